# Optimizing a Trainium2 kernel written in Bass

```python
import math
import numpy as np
import jax
import jax.numpy as jnp
from jax import lax

D_MODEL = 1024
BATCH = 16
SEQ = 2048
DEPTH = 4

CTX_LEN = 256
GRID_W = 64
HEAD_DIM = 64
ROPE_FREQS = HEAD_DIM // 4
ROPE_THETA = 10000.0
Q_BLOCK = 128
EPS = 1e-6
MOD_SCALE = 0.02

A_HEADS = 4
A_VDIM = 2 * HEAD_DIM
B_HEADS = 4
B_DK = 64
B_DV = 128
GATE_RANK = 16
GATE_TAU = 16.0
GLA_CHUNK = 64
EVEN_SPLITS = (A_HEADS * 2 * HEAD_DIM, A_HEADS * 2 * HEAD_DIM, A_HEADS * A_VDIM,
               B_HEADS * B_DK, B_HEADS * B_DK, B_HEADS * B_DV, B_HEADS * B_DV,
               GATE_RANK, GATE_RANK)
EVEN_IN = sum(EVEN_SPLITS)
EVEN_MIX = A_HEADS * A_VDIM + B_HEADS * B_DV

C_Q_HEADS = 16
C_KV_HEADS = 4
C_GROUP = C_Q_HEADS // C_KV_HEADS
ODD_SPLITS = (C_Q_HEADS * HEAD_DIM, C_KV_HEADS * HEAD_DIM, C_KV_HEADS * HEAD_DIM)
ODD_IN = sum(ODD_SPLITS)
C_WIDTH = C_Q_HEADS * HEAD_DIM

PEER_HEADS = 8
PEER_NKEYS = 128
PEER_EXPERTS = PEER_NKEYS * PEER_NKEYS
PEER_DK = 128
PEER_TOPK = 16
PEER_CHUNK = 128
PEER_V_SCALE = PEER_HEADS ** -0.5

N_EVEN = (DEPTH + 1) // 2
N_ODD = DEPTH // 2

kernel_name = 'hybrid_diffattn_gla_gqa_peer_dit'


def rms_norm(x, g):
    xf = x.astype(jnp.float32)
    y = xf * lax.rsqrt(jnp.mean(xf * xf, axis=-1, keepdims=True) + EPS)
    return (y * g.astype(jnp.float32)).astype(x.dtype)


def modulate(h, shift, scale):
    return h * (1.0 + scale) + shift


def _split(p, sizes):
    return jnp.split(p, np.cumsum(sizes)[:-1].tolist(), axis=-1)


def _heads(t, n, d):
    b, l, _ = t.shape
    return t.reshape(b, l, n, d).transpose(0, 2, 1, 3)


def _merge(t):
    b, n, l, d = t.shape
    return t.transpose(0, 2, 1, 3).reshape(b, l, n * d)


def _flip(t):
    return jnp.flip(t, axis=2)


def axial_rope_tables(n_tokens):
    rows = n_tokens // GRID_W
    row = jnp.repeat(jnp.arange(rows, dtype=jnp.float32), GRID_W)
    col = jnp.tile(jnp.arange(GRID_W, dtype=jnp.float32), rows)
    inv_freq = ROPE_THETA ** (-jnp.arange(ROPE_FREQS, dtype=jnp.float32) / ROPE_FREQS)
    ang = jnp.stack([row, col], axis=-1)[:, :, None] * inv_freq
    return jnp.cos(ang), jnp.sin(ang)


def apply_axial_rope(t, cos, sin):
    ts = t.reshape(t.shape[:-1] + (2, 2, ROPE_FREQS))
    t1, t2 = ts[..., 0, :], ts[..., 1, :]
    cos = cos.astype(t.dtype)
    sin = sin.astype(t.dtype)
    return jnp.stack([t1 * cos - t2 * sin, t2 * cos + t1 * sin], axis=-2).reshape(t.shape)


def sweep_query_blocks(fn, q, axis):
    nb = q.shape[axis] // Q_BLOCK
    qb = q.reshape(q.shape[:axis] + (nb, Q_BLOCK) + q.shape[axis + 1:])
    ob = lax.map(fn, jnp.moveaxis(qb, axis, 0))
    ob = jnp.moveaxis(ob, 0, axis)
    return ob.reshape(ob.shape[:axis] + (nb * Q_BLOCK,) + ob.shape[axis + 2:])


def diff_attend(q, k, v, lam):
    s = jnp.einsum('bhqmd,bhmkd->bhmqk', q, k).astype(jnp.float32) * HEAD_DIM ** -0.5
    p = jax.nn.softmax(s, axis=-1)
    w = p[:, :, 0] - lam * p[:, :, 1]
    return jnp.einsum('bhqk,bhkv->bhqv', w.astype(v.dtype), v)


def gqa_attend(q, k, v):
    s = jnp.einsum('bkgqd,bkld->bkgql', q, k).astype(jnp.float32) * HEAD_DIM ** -0.5
    p = jax.nn.softmax(s, axis=-1)
    return jnp.einsum('bkgql,bkld->bkgqd', p.astype(v.dtype), v)


def gla_chunked(q, k, v, log_a, h0):
    b, h, l, dk = q.shape
    dv = v.shape[-1]
    nc = l // GLA_CHUNK
    q, k, log_a = (t.reshape(b, h, nc, GLA_CHUNK, dk) for t in (q, k, log_a))
    v = v.reshape(b, h, nc, GLA_CHUNK, dv)
    cum = jnp.cumsum(log_a, axis=3)
    last = cum[:, :, :, -1:, :]
    q_dec = q * jnp.exp(cum)
    k_inv = k * jnp.exp(-cum)
    k_end = k * jnp.exp(last - cum)
    tri = jnp.tril(jnp.ones((GLA_CHUNK, GLA_CHUNK), dtype=bool))
    a = jnp.where(tri, jnp.einsum('bhncd,bhnsd->bhncs', q_dec, k_inv), 0.0)
    o_intra = jnp.einsum('bhncs,bhnsv->bhncv', a, v)
    d_state = jnp.einsum('bhncd,bhncv->bhndv', k_end, v)
    decay = jnp.exp(last[:, :, :, 0, :])

    def step(state, inp):
        g, ds = inp
        return g[..., None] * state + ds, state

    h_fin, h_in = lax.scan(step, h0, (jnp.moveaxis(decay, 2, 0), jnp.moveaxis(d_state, 2, 0)))
    o_inter = jnp.einsum('bhncd,bhndv->bhncv', q_dec, jnp.moveaxis(h_in, 0, 2))
    return (o_intra + o_inter).reshape(b, h, l, dv), h_fin


def even_mixer(a_x, a_c, cos, sin, w_in, w_out, q_gain, k_gain, lam_vec, subln,
               w_af, b_af, w_ab, b_ab, gla_gain, lam_init, need_ctx):
    b = a_x.shape[0]
    px = _split(a_x @ w_in, EVEN_SPLITS)
    pc = _split(a_c @ w_in, EVEN_SPLITS)
    lv = lam_vec.astype(jnp.float32)
    lam = jnp.exp(jnp.sum(lv[0] * lv[1])) - jnp.exp(jnp.sum(lv[2] * lv[3])) + lam_init

    def diff_qkv(p, rope):
        q = rms_norm(_heads(p[0], 2 * A_HEADS, HEAD_DIM), q_gain)
        k = rms_norm(_heads(p[1], 2 * A_HEADS, HEAD_DIM), k_gain)
        if rope:
            q = apply_axial_rope(q, cos, sin)
            k = apply_axial_rope(k, cos, sin)
        l = q.shape[2]
        q = q.reshape(b, A_HEADS, 2, l, HEAD_DIM).transpose(0, 1, 3, 2, 4)
        k = k.reshape(b, A_HEADS, 2, l, HEAD_DIM)
        v = _heads(p[2], A_HEADS, A_VDIM)
        return q, k, v

    q_x, k_x, v_x = diff_qkv(px, True)
    q_c, k_c, v_c = diff_qkv(pc, False)
    k_all = jnp.concatenate([k_c, k_x], axis=3)
    v_all = jnp.concatenate([v_c, v_x], axis=2)

    def diff_post(o):
        return _merge(rms_norm(o, subln) * (1.0 - lam_init))

    d_x = diff_post(sweep_query_blocks(lambda qb: diff_attend(qb, k_all, v_all, lam), q_x, 2))

    def gla_in(p):
        q = _heads(p[3], B_HEADS, B_DK).astype(jnp.float32) * B_DK ** -0.5
        k = _heads(p[4], B_HEADS, B_DK).astype(jnp.float32)
        v = _heads(p[5], B_HEADS, B_DV).astype(jnp.float32)
        la_f = _heads(jax.nn.log_sigmoid((p[7] @ w_af + b_af).astype(jnp.float32)) / GATE_TAU, B_HEADS, B_DK)
        la_b = _heads(jax.nn.log_sigmoid((p[8] @ w_ab + b_ab).astype(jnp.float32)) / GATE_TAU, B_HEADS, B_DK)
        return q, k, v, la_f, la_b

    gq_x, gk_x, gv_x, gf_x, gb_x = gla_in(px)
    gq_c, gk_c, gv_c, gf_c, gb_c = gla_in(pc)
    zeros = jnp.zeros((b, B_HEADS, B_DK, B_DV), jnp.float32)
    oc_f, hc_f = gla_chunked(gq_c, gk_c, gv_c, gf_c, zeros)
    oc_b, hc_b = gla_chunked(_flip(gq_c), _flip(gk_c), _flip(gv_c), _flip(gb_c), zeros)
    ox_f, _ = gla_chunked(gq_x, gk_x, gv_x, gf_x, hc_f)
    ox_b, _ = gla_chunked(_flip(gq_x), _flip(gk_x), _flip(gv_x), _flip(gb_x), hc_b)

    def gla_post(o, r):
        return _merge(rms_norm(o, gla_gain).astype(r.dtype)) * jax.nn.silu(r)

    g_x = gla_post(ox_f + _flip(ox_b), px[6])
    y_x = jnp.concatenate([d_x, g_x], axis=-1) @ w_out
    y_c = None
    if need_ctx:
        d_c = diff_post(diff_attend(q_c, k_c, v_c, lam))
        g_c = gla_post(oc_f + _flip(oc_b), pc[6])
        y_c = jnp.concatenate([d_c, g_c], axis=-1) @ w_out
    return y_x, y_c


def odd_mixer(a_x, a_c, cos, sin, w_in, w_out, q_gain, k_gain, need_ctx):
    q_cols = ODD_SPLITS[0]

    def qkv(a, with_q, rope):
        if with_q:
            pq, pk, pv = _split(a @ w_in, ODD_SPLITS)
        else:
            pk, pv = _split(a @ w_in[:, q_cols:], ODD_SPLITS[1:])
        k = rms_norm(_heads(pk, C_KV_HEADS, HEAD_DIM), k_gain)
        v = _heads(pv, C_KV_HEADS, HEAD_DIM)
        if rope:
            k = apply_axial_rope(k, cos, sin)
        q = None
        if with_q:
            q = rms_norm(_heads(pq, C_Q_HEADS, HEAD_DIM), q_gain)
            if rope:
                q = apply_axial_rope(q, cos, sin)
            bq, _, l, _ = q.shape
            q = q.reshape(bq, C_KV_HEADS, C_GROUP, l, HEAD_DIM)
        return q, k, v

    q_x, k_x, v_x = qkv(a_x, True, True)
    q_c, k_c, v_c = qkv(a_c, need_ctx, False)
    k_all = jnp.concatenate([k_c, k_x], axis=2)
    v_all = jnp.concatenate([v_c, v_x], axis=2)

    def post(o):
        bo, _, _, l, _ = o.shape
        return _merge(o.reshape(bo, C_Q_HEADS, l, HEAD_DIM)) @ w_out

    y_x = post(sweep_query_blocks(lambda qb: gqa_attend(qb, k_all, v_all), q_x, 3))
    y_c = post(gqa_attend(q_c, k_c, v_c)) if need_ctx else None
    return y_x, y_c


def peer_ffn(h, w_q, sub_keys, u_tab, v_tab):
    b, l, d = h.shape
    half = PEER_DK // 2

    def chunk(xc):
        q = (xc @ w_q).reshape(PEER_CHUNK, PEER_HEADS, PEER_DK)
        s1 = jnp.einsum('thd,hnd->thn', q[..., :half], sub_keys[:, 0]).astype(jnp.float32)
        s2 = jnp.einsum('thd,hnd->thn', q[..., half:], sub_keys[:, 1]).astype(jnp.float32)
        v1, i1 = lax.top_k(s1, PEER_TOPK)
        v2, i2 = lax.top_k(s2, PEER_TOPK)
        cand = (v1[..., :, None] + v2[..., None, :]).reshape(PEER_CHUNK, PEER_HEADS, PEER_TOPK * PEER_TOPK)
        cidx = (i1[..., :, None] * PEER_NKEYS + i2[..., None, :]).reshape(PEER_CHUNK, PEER_HEADS, PEER_TOPK * PEER_TOPK)
        best, j = lax.top_k(cand, PEER_TOPK)
        idx = jnp.take_along_axis(cidx, j, axis=-1)
        g = jax.nn.softmax(best, axis=-1)
        act = jax.nn.gelu(jnp.einsum('thkd,td->thk', u_tab[idx], xc).astype(jnp.float32))
        w = (g * act).astype(xc.dtype)
        return jnp.einsum('thk,thkd->td', w, v_tab[idx])

    out = lax.map(chunk, h.reshape(-1, PEER_CHUNK, d))
    return out.reshape(b, l, d)


def setup_inputs(seed: int = 0) -> dict:
    key = jax.random.key(seed)
    ks = jax.random.split(key, 30)

    def nrm(k, shape, s):
        return jax.random.normal(k, shape, jnp.float32) * s

    D = D_MODEL
    return {
        'x': nrm(ks[0], (BATCH, SEQ, D), 1.0),
        'c': nrm(ks[1], (BATCH, D), 1.0),
        'ctx': nrm(ks[2], (BATCH, CTX_LEN, D), 1.0),
        'c_ctx': nrm(ks[3], (D,), 1.0),
        'w_mod': nrm(ks[4], (DEPTH, D, 6 * D), MOD_SCALE),
        'b_mod': nrm(ks[5], (DEPTH, 6 * D), 0.01),
        'g_mix': 1.0 + nrm(ks[6], (DEPTH, D), 0.02),
        'g_ffn': 1.0 + nrm(ks[7], (DEPTH, D), 0.02),
        'w_in_even': nrm(ks[8], (N_EVEN, D, EVEN_IN), D ** -0.5),
        'w_out_even': nrm(ks[9], (N_EVEN, EVEN_MIX, D), EVEN_MIX ** -0.5),
        'a_q_gain': 1.0 + nrm(ks[10], (N_EVEN, HEAD_DIM), 0.02),
        'a_k_gain': 1.0 + nrm(ks[11], (N_EVEN, HEAD_DIM), 0.02),
        'a_lambda': nrm(ks[12], (N_EVEN, 4, HEAD_DIM), 0.1),
        'a_subln': 1.0 + nrm(ks[13], (N_EVEN, A_VDIM), 0.02),
        'b_w_af': nrm(ks[14], (N_EVEN, GATE_RANK, B_HEADS * B_DK), GATE_RANK ** -0.5),
        'b_b_af': 1.0 + nrm(ks[15], (N_EVEN, B_HEADS * B_DK), 0.5),
        'b_w_ab': nrm(ks[16], (N_EVEN, GATE_RANK, B_HEADS * B_DK), GATE_RANK ** -0.5),
        'b_b_ab': 1.0 + nrm(ks[17], (N_EVEN, B_HEADS * B_DK), 0.5),
        'b_gain': 1.0 + nrm(ks[18], (N_EVEN, B_DV), 0.02),
        'w_in_odd': nrm(ks[19], (N_ODD, D, ODD_IN), D ** -0.5),
        'w_out_odd': nrm(ks[20], (N_ODD, C_WIDTH, D), C_WIDTH ** -0.5),
        'c_q_gain': 1.0 + nrm(ks[21], (N_ODD, HEAD_DIM), 0.02),
        'c_k_gain': 1.0 + nrm(ks[22], (N_ODD, HEAD_DIM), 0.02),
        'peer_wq': nrm(ks[23], (DEPTH, D, PEER_HEADS * PEER_DK), D ** -0.5),
        'peer_keys': nrm(ks[24], (DEPTH, PEER_HEADS, 2, PEER_NKEYS, PEER_DK // 2), (PEER_DK // 2) ** -0.5),
        'peer_u': nrm(ks[25], (DEPTH, PEER_EXPERTS, D), D ** -0.5),
        'peer_v': nrm(ks[26], (DEPTH, PEER_EXPERTS, D), PEER_V_SCALE),
    }


def reference(x, c, ctx, c_ctx, w_mod, b_mod, g_mix, g_ffn,
              w_in_even, w_out_even, a_q_gain, a_k_gain, a_lambda, a_subln,
              b_w_af, b_b_af, b_w_ab, b_b_ab, b_gain,
              w_in_odd, w_out_odd, c_q_gain, c_k_gain,
              peer_wq, peer_keys, peer_u, peer_v):
    cos, sin = axial_rope_tables(x.shape[1])
    h_c = ctx
    silu_c = jax.nn.silu(c)
    silu_cc = jax.nn.silu(c_ctx)[None, :]
    for layer in range(DEPTH):
        need_ctx = layer < DEPTH - 1
        mx = jnp.split((silu_c @ w_mod[layer] + b_mod[layer])[:, None, :], 6, axis=-1)
        mc = jnp.split((silu_cc @ w_mod[layer] + b_mod[layer])[:, None, :], 6, axis=-1)
        a_x = modulate(rms_norm(x, g_mix[layer]), mx[0], mx[1])
        a_c = modulate(rms_norm(h_c, g_mix[layer]), mc[0], mc[1])
        i = layer // 2
        if layer % 2 == 0:
            lam_init = 0.8 - 0.6 * math.exp(-0.3 * layer)
            y_x, y_c = even_mixer(a_x, a_c, cos, sin, w_in_even[i], w_out_even[i],
                                  a_q_gain[i], a_k_gain[i], a_lambda[i], a_subln[i],
                                  b_w_af[i], b_b_af[i], b_w_ab[i], b_b_ab[i], b_gain[i],
                                  lam_init, need_ctx)
        else:
            y_x, y_c = odd_mixer(a_x, a_c, cos, sin, w_in_odd[i], w_out_odd[i],
                                 c_q_gain[i], c_k_gain[i], need_ctx)
        x = x + mx[2] * y_x
        f_x = peer_ffn(modulate(rms_norm(x, g_ffn[layer]), mx[3], mx[4]),
                       peer_wq[layer], peer_keys[layer], peer_u[layer], peer_v[layer])
        x = x + mx[5] * f_x
        if need_ctx:
            h_c = h_c + mc[2] * y_c
            f_c = peer_ffn(modulate(rms_norm(h_c, g_ffn[layer]), mc[3], mc[4]),
                           peer_wq[layer], peer_keys[layer], peer_u[layer], peer_v[layer])
            h_c = h_c + mc[5] * f_c
    return x
```

```python
import math
import numpy as np
import concourse.bass as bass
import concourse.mybir as mybir
from concourse.bass_utils import run_bass_kernel_spmd

F32 = mybir.dt.float32
U32 = mybir.dt.uint32
F32R = mybir.dt.float32r
BF16 = mybir.dt.bfloat16


def _r(ap):
    return ap.bitcast(F32R)
AF = mybir.ActivationFunctionType
ALU = mybir.AluOpType
AX = mybir.AxisListType

D = 1024
DEPTH = 4
NB = 2
CTX = 256
SEQ = 2048
T = CTX + SEQ
NT = T // 128
NCT = CTX // 128
HD = 64
GRID_W = 64
EPS = 1e-6
NEXP = 16384
NEG = -1.0e30


class _Op:
    __slots__ = ("eng", "fn", "reads", "writes", "dma", "idx", "stream", "spos",
                 "waits", "signal", "sigval", "slot")


class Sched:
    ENGS = ("sp", "act", "pool", "pe", "dve")

    def __init__(self, nc, esem, dsem, dbase=None):
        self.nc = nc
        self.esem = esem
        self.dsem = dsem
        self.dbase = dbase if dbase is not None else {}
        self.ops = []

    def add(self, eng, fn, reads=(), writes=(), dma=False, slot=None):
        op = _Op()
        op.eng, op.fn, op.reads, op.writes, op.dma = eng, fn, tuple(reads), tuple(writes), dma
        op.slot = slot
        op.idx = len(self.ops)
        op.signal = False
        op.waits = ()
        self.ops.append(op)
        return op

    def _analyse(self):
        ops = self.ops
        last_w, readers = {}, {}
        pos = {e: 0 for e in self.ENGS}
        dcnt = {e: self.dbase.get(e, 0) for e in self.ENGS}
        slot_last = {}
        slot_last = {}
        clock = {e: {} for e in self.ENGS}
        for op in ops:
            deps = set()
            for r in op.reads:
                w = last_w.get(r)
                if w is not None:
                    deps.add(w)
            for k in op.writes:
                w = last_w.get(k)
                if w is not None:
                    deps.add(w)
                deps.update(readers.get(k, ()))
            for k in op.writes:
                last_w[k] = op.idx
                readers[k] = []
            for r in op.reads:
                readers.setdefault(r, []).append(op.idx)
            deps.discard(op.idx)
            if op.dma and op.slot is not None:
                key = (op.eng, op.slot)
                j = self.dbase.get(key, 0)
                self.dbase[key] = j + 1
                op.stream = key
                op.spos = j
                prev = slot_last.get(op.stream)
                if prev is not None:
                    deps.add(prev)
                slot_last[op.stream] = op.idx
            elif op.dma:
                K = len(self.dsem[op.eng])
                j = dcnt[op.eng]
                dcnt[op.eng] += 1
                op.stream = (op.eng, j % K)
                op.spos = j // K
                prev = slot_last.get(op.stream)
                if prev is not None:
                    deps.add(prev)
                slot_last[op.stream] = op.idx
            else:
                op.stream = op.eng
                op.spos = pos[op.eng]
            pos[op.eng] += 1
            need = {}
            implied = {}
            for d in deps:
                P = ops[d]
                if (not P.dma) and (not op.dma) and P.eng == "pe" and op.eng == "pe":
                    continue
                if need.get(P.stream, (-1, None))[0] < P.spos:
                    need[P.stream] = (P.spos, d)
                for q in P.waits:
                    Q = ops[q]
                    if implied.get(Q.stream, -1) < Q.spos:
                        implied[Q.stream] = Q.spos
            ck = clock[op.eng]
            waits = []
            for stream, (sp_, d) in need.items():
                if ck.get(stream, -1) >= sp_:
                    continue
                if implied.get(stream, -1) >= sp_:
                    continue
                waits.append(d)
                ops[d].signal = True
            for stream, (sp_, d) in need.items():
                if ck.get(stream, -1) < sp_:
                    ck[stream] = sp_
            for stream, sp_ in implied.items():
                if ck.get(stream, -1) < sp_:
                    ck[stream] = sp_
            op.waits = tuple(sorted(waits))
        tails = {}
        for op in ops:
            tails[op.stream] = op.idx
        self.fence = []
        for stream, d in tails.items():
            ops[d].signal = True
            self.fence.append(d)
        cnt = {e: 0 for e in self.ENGS}
        for op in ops:
            if op.dma:
                op.signal = True
                op.sigval = 16 * (op.spos + 1)
            elif op.signal:
                cnt[op.eng] += 1
                op.sigval = cnt[op.eng]
        for e in list(self.dbase):
            if not isinstance(e, tuple):
                self.dbase[e] = dcnt[e]

    def _sem_of(self, P):
        if P.dma:
            return self.dsem[P.stream[0]][P.stream[1]]
        return self.esem[P.eng]

    def emit(self, block):
        self._analyse()
        ops = self.ops
        sched = self

        def run(eng_name):
            def body(e):
                for op in ops:
                    if op.eng != eng_name:
                        continue
                    for d in op.waits:
                        P = ops[d]
                        e.wait_ge(sched._sem_of(P), P.sigval)
                    ins = op.fn(e)
                    if op.dma:
                        ins.then_inc(sched.dsem[op.stream[0]][op.stream[1]], 16)
                    elif op.signal:
                        ins.then_inc(sched.esem[op.eng], 1)
                if eng_name == "sp":
                    for d in sched.fence:
                        P = ops[d]
                        e.wait_ge(sched._sem_of(P), P.sigval)
            return body

        block.sync(run("sp"))
        block.scalar(run("act"))
        block.gpsimd(run("pool"))
        block.tensor(run("pe"))
        block.vector(run("dve"))
        self.ops = []


class Builder:
    def __init__(self, nc, cfg):
        self.nc = nc
        self.cfg = cfg
        self.dram = {}

    def din(self, name, shape, dt=F32):
        t = self.nc.dram_tensor(name, list(shape), dt, kind="ExternalInput").ap()
        self.dram[name] = t
        return t

    def dout(self, name, shape, dt=F32):
        t = self.nc.dram_tensor(name, list(shape), dt, kind="ExternalOutput").ap()
        self.dram[name] = t
        return t

    def dscr(self, name, shape, dt=F32):
        kind = "ExternalOutput" if name in self.cfg.get("dump", ()) else "Internal"
        t = self.nc.dram_tensor(name, list(shape), dt, kind=kind).ap()
        self.dram[name] = t
        return t

    def phase(self, fn):
        nc = self.nc
        with nc.Block(no_gpsimd_drain=True) as blk:
            sems = list(self.esem.values()) + [s for k_, v in self.dsem.items() if k_ != "pool" for s in v]

            def clr(e):
                for s in sems:
                    e.sem_clear(s)
            blk.sync(clr)
        with nc.Block(no_gpsimd_drain=True) as blk:
            if not hasattr(self, "dbase"):
                self.dbase = {"pool": 0}
            self.s = Sched(nc, self.esem, self.dsem, self.dbase)
            fn()
            self.s.emit(blk)

    def add(self, eng, fn, r=(), w=(), dma=False):
        return self.s.add(eng, fn, r, w, dma)

    def load(self, out, in_, r, w, eng="sp"):
        return self.s.add(eng, lambda e: e.dma_start(out=out, in_=in_), r, w, dma=True)

    def op(self, eng, name, r, w, *a, **k):
        return self.s.add(eng, lambda e: getattr(e, name)(*a, **k), r, w)

    def dma(self, eng, out, in_, r, w):
        return self.s.add(eng, lambda e: e.dma_start(out=out, in_=in_), r, w, dma=True)

    def _uname(self, name):
        self._uid = getattr(self, "_uid", 0) + 1
        return f"{name}_{self._uid}"

    def sb(self, st, name, shape, dt=F32):
        return st.enter_context(self.nc.sbuf_tensor(self._uname("s_" + name), list(shape), dt))

    def ps(self, st, name, shape, dt=F32):
        return st.enter_context(self.nc.psum_tensor(self._uname("p_" + name), list(shape), dt))

    def phase_mod(self, layers):
        from contextlib import ExitStack
        d = self.dram
        with ExitStack() as st:
            cT = self.sb(st, "cT", [128, 8, 3])
            bm = self.sb(st, "bm", [3, 6 * D])
            gm = self.sb(st, "gm", [3, D])
            gf = self.sb(st, "gf", [3, D])
            msb = self.sb(st, "msb", [3, 6 * D])
            wb = [self.sb(st, f"wmb{i}", [128, 8, 512]) for i in range(2)]
            mp = [self.ps(st, f"mp{i}", [3, 512]) for i in range(2)]

            def body():
                self.dma("sp", cT[:], d["cT3"][:, :, :], [], ["cT"])
                self.op("act", "activation", ["cT"], ["cT"], out=cT[:], in_=cT[:], func=AF.Silu)
                for l in layers:
                    self.dma("sp", bm[:], d["b_mod"][l:l + 1, :].to_broadcast([3, 6 * D]), [], ["bm"])
                    self.dma("sp", gm[:], d["g_mix"][l:l + 1, :].to_broadcast([3, D]), [], ["gm"])
                    self.dma("sp", gf[:], d["g_ffn"][l:l + 1, :].to_broadcast([3, D]), [], ["gf"])
                    for n in range(12):
                        w_ = wb[n % 2]
                        src = d["w_mod"][l, :, n * 512:(n + 1) * 512].rearrange("(k p) n -> p k n", p=128)
                        self.dma("sp" if n % 2 == 0 else "act", w_[:], src, [], [("wmb", n % 2)])
                        for k in range(8):
                            self.op("pe", "matmul", ["cT", ("wmb", n % 2)], [("mp", n % 2)],
                                    mp[n % 2][:, :], cT[:, k, :], w_[:, k, :], start=(k == 0), stop=(k == 7))
                        self.op("dve", "tensor_tensor", [("mp", n % 2), "bm"], [("msb", n)],
                                out=msb[:, n * 512:(n + 1) * 512], in0=mp[n % 2][:, :],
                                in1=bm[:, n * 512:(n + 1) * 512], op=ALU.add)
                    for (c0, gt, gk) in ((1, gm, "gm"), (4, gf, "gf")):
                        ks = [("msb", 2 * c0), ("msb", 2 * c0 + 1)]
                        sl = msb[:, c0 * D:(c0 + 1) * D]
                        self.op("dve", "tensor_scalar", ks, ks, out=sl, in0=sl, scalar1=1.0, scalar2=None,
                                op0=ALU.add)
                        self.op("dve", "tensor_tensor", ks + [gk], ks, out=sl, in0=sl, in1=gt[:], op=ALU.mult)
                    self.dma("sp", d["modD"][l, :, :], msb[:], [("msb", n) for n in range(12)], [("modD", l)])
            self.phase(body)

    def load_mod(self, dst, l, who, j, key, eng="sp"):
        src = self.dram["modD"][l, who:who + 1, j * D:(j + 1) * D].to_broadcast([128, D])
        self.dma(eng, dst[:], src, [], [key])

    def phase_p3(self, l, tiles, xsrc="xs"):
        from contextlib import ExitStack
        d = self.dram
        w_out = d["w_out_even"][l // 2] if l % 2 == 0 else d["w_out_odd"][l // 2]
        with ExitStack() as st:
            wo = self.sb(st, "wo", [128, 8, D])
            wq = self.sb(st, "wq", [128, 8, D])
            kbd = self.sb(st, "kbd", [128, 8, 256])
            ident = self.sb(st, "ident", [128, 128])
            g2 = self.sb(st, "g2", [128, D])
            shf = self.sb(st, "shf", [128, D])
            gsf = self.sb(st, "gsf", [128, D])
            Mt = [self.sb(st, f"Mt{i}", [128, D]) for i in range(2)]
            xt = [self.sb(st, f"xt{i}", [128, D]) for i in range(2)]
            x1 = [self.sb(st, f"x1{i}", [128, D]) for i in range(2)]
            a2 = [self.sb(st, f"a2{i}", [128, D]) for i in range(2)]
            Ssb = [self.sb(st, f"Ssb{i}", [128, 2048]) for i in range(2)]
            MT = self.sb(st, "MT", [128, D])
            a2T = self.sb(st, "a2T", [128, D])
            q = self.sb(st, "q", [128, D])
            qT = self.sb(st, "qT", [128, D])
            junk = self.sb(st, "junk", [128, D])
            ss = self.sb(st, "ss", [128, 2])
            tp = self.ps(st, "tp", [128, D])
            yp = self.ps(st, "yp", [128, D])
            spm = self.ps(st, "spm", [128, 2048])

            def body():
                for k in range(8):
                    self.dma("sp", junk[:], w_out[k * 128:(k + 1) * 128, :], [], ["junk"])
                    self.op("dve", "tensor_copy", ["junk"], ["wo"], out=_r(wo[:, k, :]), in_=junk[:])
                for k in range(8):
                    self.dma("act", q[:], d["peer_wq"][l, k * 128:(k + 1) * 128, :], [], ["q"])
                    self.op("dve", "tensor_copy", ["q"], ["wq"], out=_r(wq[:, k, :]), in_=q[:])
                self.dma("sp", Ssb[0][:, :].rearrange("p (h n) -> p h n", h=8), d["kbd"][l], [], [("Ssb", 0)])
                self.op("dve", "tensor_copy", [("Ssb", 0)], ["kbd"], out=_r(kbd[:]),
                        in_=Ssb[0][:, :].rearrange("p (h n) -> p h n", h=8))
                self.dma("sp", ident[:], d["ident"][:, :], [], ["ident"])
                cur_who = None
                for i, (b, tl) in enumerate(tiles):
                    par = i % 2
                    who = 2 if tl < NCT else b
                    rows = slice(tl * 128, (tl + 1) * 128)
                    if who != cur_who:
                        cur_who = who
                        self.load_mod(g2, l, who, 2, "g2")
                        self.load_mod(shf, l, who, 3, "shf")
                        self.load_mod(gsf, l, who, 4, "gsf")
                    kM, kx, kx1, ka2, kS = ("Mt", par), ("xt", par), ("x1", par), ("a2", par), ("Ssb", par)
                    self.dma("sp", Mt[par][:], d["Mx"][b, rows, :], [("Mx", b, tl)], [kM])
                    self.dma("act", xt[par][:], d[xsrc][b, rows, :], [("xs", b, tl)], [kx])
                    for k in range(8):
                        cs = slice(k * 128, (k + 1) * 128)
                        self.op("pe", "transpose", [kM, "ident"], ["tp"], out=tp[:, cs], in_=Mt[par][:, cs],
                                identity=ident[:])
                    self.op("dve", "tensor_copy", ["tp"], ["MT"], out=_r(MT[:]), in_=tp[:])
                    for n in range(2):
                        ns = slice(n * 512, (n + 1) * 512)
                        for k in range(8):
                            self.op("pe", "matmul", ["MT", "wo"], ["yp"], yp[:, ns], _r(MT[:, k * 128:(k + 1) * 128]),
                                    _r(wo[:, k, ns]), start=(k == 0), stop=(k == 7))
                    self.op("dve", "tensor_tensor", ["yp", "g2"], [kx1], out=x1[par][:], in0=yp[:], in1=g2[:],
                            op=ALU.mult)
                    self.op("dve", "tensor_tensor", [kx1, kx], [kx1], out=x1[par][:], in0=x1[par][:],
                            in1=xt[par][:], op=ALU.add)
                    self.dma("sp", d["xs"][b, rows, :], x1[par][:], [kx1], [("xs", b, tl)])
                    self.op("act", "activation", [kx1], ["junk", "ss"], out=junk[:], in_=x1[par][:], func=AF.Square,
                            accum_out=ss[:, 0:1])
                    self.op("dve", "tensor_scalar", ["ss"], ["ss"], out=ss[:, 0:1], in0=ss[:, 0:1],
                            scalar1=1.0 / D, scalar2=EPS, op0=ALU.mult, op1=ALU.add)
                    self.op("act", "activation", ["ss"], ["ss"], out=ss[:, 0:1], in_=ss[:, 0:1], func=AF.Sqrt)
                    self.op("dve", "reciprocal", ["ss"], ["ss"], out=ss[:, 0:1], in_=ss[:, 0:1])
                    self.op("dve", "scalar_tensor_tensor", [kx1, "ss", "gsf"], [ka2], out=a2[par][:],
                            in0=x1[par][:], scalar=ss[:, 0:1], in1=gsf[:], op0=ALU.mult, op1=ALU.mult)
                    self.op("dve", "tensor_tensor", [ka2, "shf"], [ka2], out=a2[par][:], in0=a2[par][:],
                            in1=shf[:], op=ALU.add)
                    self.dma("sp", d["A2"][b, rows, :], a2[par][:], [ka2], [("A2", b, tl)])
                    for k in range(8):
                        cs = slice(k * 128, (k + 1) * 128)
                        self.op("pe", "transpose", [ka2, "ident"], ["tp"], out=tp[:, cs], in_=a2[par][:, cs],
                                identity=ident[:])
                    self.op("dve", "tensor_copy", ["tp"], ["a2T"], out=_r(a2T[:]), in_=tp[:])
                    for n in range(2):
                        ns = slice(n * 512, (n + 1) * 512)
                        for k in range(8):
                            self.op("pe", "matmul", ["a2T", "wq"], ["yp"], yp[:, ns], _r(a2T[:, k * 128:(k + 1) * 128]),
                                    _r(wq[:, k, ns]), start=(k == 0), stop=(k == 7))
                    self.op("act", "copy", ["yp"], ["q"], out=q[:], in_=yp[:])
                    for k in range(8):
                        cs = slice(k * 128, (k + 1) * 128)
                        self.op("pe", "transpose", ["q", "ident"], ["tp"], out=tp[:, cs], in_=q[:, cs],
                                identity=ident[:])
                    self.op("dve", "tensor_copy", ["tp"], ["qT"], out=_r(qT[:]), in_=tp[:])
                    for h in range(8):
                        self.op("pe", "matmul", ["qT", "kbd"], ["spm"], spm[:, h * 256:(h + 1) * 256],
                                _r(qT[:, h * 128:(h + 1) * 128]), _r(kbd[:, h, :]), start=True, stop=True)
                    self.op("act", "copy", ["spm"], [kS], out=Ssb[par][:, 0:1024], in_=spm[:, 0:1024])
                    self.op("dve", "tensor_copy", ["spm"], [kS], out=Ssb[par][:, 1024:2048], in_=spm[:, 1024:2048])
                    self.dma("sp", d["Sc"][b, rows, :], Ssb[par][:], [kS], [("Sc", b, tl)])
            self.phase(body)

    def phase_p4(self, l, tiles, final):
        from contextlib import ExitStack
        d = self.dram
        NS = 6
        C0 = 0.7978845608028654
        with ExitStack() as st:
            S = [self.sb(st, f"S{i}", [128, 2048]) for i in range(3)]
            a2 = [self.sb(st, f"pa2{i}", [128, D]) for i in range(3)]
            x1 = [self.sb(st, f"px1{i}", [128, D]) for i in range(3)]
            idx = [self.sb(st, f"idx{i}", [128, 128], U32) for i in range(3)]
            wgt = [self.sb(st, f"wgt{i}", [128, 128]) for i in range(3)]
            acc = [self.sb(st, f"acc{i}", [128, D]) for i in range(3)]
            S2 = self.sb(st, "S2", [128, 2048])
            V16 = self.sb(st, "V16", [128, 256])
            I16 = self.sb(st, "I16", [128, 256], U32)
            I16f = self.sb(st, "I16f", [128, 256])
            cand = self.sb(st, "cand", [128, 2048])
            cand2 = self.sb(st, "cand2", [128, 2048])
            cidx = self.sb(st, "cidx", [128, 2048])
            best = self.sb(st, "best", [128, 128])
            J = self.sb(st, "J", [128, 128], U32)
            Jf = self.sb(st, "Jf", [128, 128])
            idxf = self.sb(st, "idxf", [128, 128])
            ex = self.sb(st, "ex", [128, 128])
            Z = self.sb(st, "Z", [128, 8])
            gte = [self.sb(st, f"gte{i}", [128, 128]) for i in range(3)]
            who_first, who_par = {}, {}
            final_ = final
            araw = self.sb(st, "araw", [128, 128])
            t1 = self.sb(st, "t1", [128, 128])
            t2 = self.sb(st, "t2", [128, 128])
            iota = self.sb(st, "iota", [128, 256])
            junk = self.ps(st, "pjunk", [128, D])
            junk2 = self.ps(st, "pjunk2", [128, 256])
            g5 = [self.sb(st, f"g5{i}", [128, D]) for i in range(2)]
            ub = [self.sb(st, f"ub{i}", [128, D]) for i in range(NS)]
            vb = [self.sb(st, f"vb{i}", [128, D]) for i in range(NS)]
            utab = d["peer_u"].rearrange("l e d -> (l e) d")
            vtab = d["peer_v"].rearrange("l e d -> (l e) d")

            def v3(t, a, b_):
                return t[:, :].rearrange("p (a b) -> p a b", a=a)

            ident = self.sb(st, "pident", [128, 128])
            dg = [self.sb(st, f"dg{i}", [128, 128]) for i in range(4)]
            accp = [self.ps(st, f"accp{i}", [128, D]) for i in range(2)]
            n_t = len(tiles)

            def keys(i):
                par = i % 3
                return ("S", par), ("pa2", par), ("px1", par), ("idx", par), ("wgt", par), ("acc", par)

            def loads(i):
                b, tl = tiles[i]
                par = i % 3
                rows = slice(tl * 128, (tl + 1) * 128)
                kS, ka2, kx1, kidx, kw, kacc = keys(i)
                self.dma("sp", S[par][:], d["Sc"][b, rows, :], [("Sc", b, tl)], [kS])
                self.dma("act", a2[par][:], d["A2"][b, rows, :], [("A2", b, tl)], [ka2])
                self.dma("act", x1[par][:], d["xs"][b, rows, :], [("xs", b, tl)], [kx1])

            def topk(i):
                par = i % 3
                kS, ka2, kx1, kidx, kw, kacc = keys(i)
                Sp = S[par]
                for g in range(16):
                    gs = slice(g * 128, (g + 1) * 128)
                    lo = slice(g * 16, g * 16 + 8)
                    hi = slice(g * 16 + 8, g * 16 + 16)
                    yield self.op("dve", "max", [kS], [("V16", g)], out=V16[:, lo], in_=Sp[:, gs])
                    yield self.op("dve", "max_index", [kS, ("V16", g)], [("I16", g)], out=I16[:, lo],
                            in_max=V16[:, lo], in_values=Sp[:, gs])
                    yield self.op("dve", "match_replace", [kS, ("V16", g)], [("S2", g)], out=S2[:, gs],
                            in_to_replace=V16[:, lo], in_values=Sp[:, gs], imm_value=NEG)
                    yield self.op("dve", "max", [("S2", g)], [("V16", g)], out=V16[:, hi], in_=S2[:, gs])
                    yield self.op("dve", "max_index", [("S2", g), ("V16", g)], [("I16", g)], out=I16[:, hi],
                            in_max=V16[:, hi], in_values=S2[:, gs])
                allV = [("V16", g) for g in range(16)]
                allI = [("I16", g) for g in range(16)]
                yield self.op("dve", "tensor_copy", allI, ["I16f"], out=I16f[:], in_=I16[:])
                I4 = I16f[:, :].rearrange("p (h t k) -> p h t k", h=8, t=2)
                V4 = V16[:, :].rearrange("p (h t k) -> p h t k", h=8, t=2)
                yield self.op("dve", "tensor_scalar", ["I16f"], ["I16f"], out=I4[:, :, 0, :], in0=I4[:, :, 0, :],
                        scalar1=128.0, scalar2=None, op0=ALU.mult)
                c4 = cand[:, :].rearrange("p (h a c) -> p h a c", h=8, a=16)
                x4 = cidx[:, :].rearrange("p (h a c) -> p h a c", h=8, a=16)
                yield self.op("dve", "tensor_tensor", allV, ["cand"], out=c4,
                        in0=V4[:, :, 0, :].unsqueeze(3).to_broadcast([128, 8, 16, 16]),
                        in1=V4[:, :, 1, :].unsqueeze(2).to_broadcast([128, 8, 16, 16]), op=ALU.add)
                yield self.op("dve", "tensor_tensor", ["I16f"], ["cidx"], out=x4,
                        in0=I4[:, :, 0, :].unsqueeze(3).to_broadcast([128, 8, 16, 16]),
                        in1=I4[:, :, 1, :].unsqueeze(2).to_broadcast([128, 8, 16, 16]), op=ALU.add)
                for h in range(8):
                    hs = slice(h * 256, (h + 1) * 256)
                    lo = slice(h * 16, h * 16 + 8)
                    hi = slice(h * 16 + 8, h * 16 + 16)
                    yield self.op("dve", "max", ["cand"], [("best", h)], out=best[:, lo], in_=cand[:, hs])
                    yield self.op("dve", "max_index", ["cand", ("best", h)], [("J", h)], out=J[:, lo],
                            in_max=best[:, lo], in_values=cand[:, hs])
                    yield self.op("dve", "match_replace", ["cand", ("best", h)], [("cand2", h)], out=cand2[:, hs],
                            in_to_replace=best[:, lo], in_values=cand[:, hs], imm_value=NEG)
                    yield self.op("dve", "max", [("cand2", h)], [("best", h)], out=best[:, hi], in_=cand2[:, hs])
                    yield self.op("dve", "max_index", [("cand2", h), ("best", h)], [("J", h)], out=J[:, hi],
                            in_max=best[:, hi], in_values=cand2[:, hs])
                allB = [("best", h) for h in range(8)]
                allJ = [("J", h) for h in range(8)]
                yield self.op("dve", "tensor_copy", allJ, ["Jf"], out=Jf[:], in_=J[:])
                for r in range(128):
                    h = r // 16
                    yield self.op("dve", "scalar_tensor_tensor", ["iota", "Jf", "cidx"], ["junk2", ("idxf", r)],
                            out=junk2[:], in0=iota[:], scalar=Jf[:, r:r + 1],
                            in1=cidx[:, h * 256:(h + 1) * 256], op0=ALU.is_equal, op1=ALU.mult,
                            accum_out=idxf[:, r:r + 1])
                allF = [("idxf", r) for r in range(128)]
                yield self.op("dve", "tensor_scalar", allF, [kidx], out=idx[par][:], in0=idxf[:],
                        scalar1=float(l * NEXP), scalar2=None, op0=ALU.add)
                b3, e3, g3 = v3(best, 8, 16), v3(ex, 8, 16), v3(gte[par], 8, 16)
                yield self.op("dve", "tensor_tensor", allB, ["ex"], out=e3, in0=b3,
                        in1=b3[:, :, 0:1].to_broadcast([128, 8, 16]), op=ALU.subtract)
                yield self.op("act", "activation", ["ex"], ["ex"], out=ex[:], in_=ex[:], func=AF.Exp)
                yield self.op("dve", "reduce_sum", ["ex"], ["Z"], out=Z[:], in_=e3, axis=AX.X)
                yield self.op("dve", "reciprocal", ["Z"], ["Z"], out=Z[:], in_=Z[:])
                yield self.op("dve", "tensor_tensor", ["ex", "Z"], [("gte", par)], out=g3, in0=e3,
                        in1=Z[:, :].unsqueeze(2).to_broadcast([128, 8, 16]), op=ALU.mult)

            def ustep(i, r):
                par = i % 3
                kS, ka2, kx1, kidx, kw, kacc = keys(i)
                if True:
                    s_ = r % NS
                    self.s.add("pool", (lambda e, o=ub[s_], ix=idx[par], r=r: e.indirect_dma_start(
                        out=o[:], out_offset=None, in_=utab,
                        in_offset=bass.IndirectOffsetOnAxis(ap=ix[:, r:r + 1], axis=0))),
                        [kidx], [("ub", s_)], dma=True, slot=s_)
                    self.op("dve", "scalar_tensor_tensor", [("ub", s_), ka2], ["junk", ("araw", r)],
                            out=junk[:], in0=ub[s_][:], scalar=1.0, in1=a2[par][:], op0=ALU.mult,
                            op1=ALU.mult, accum_out=araw[:, r:r + 1])

            def gelu(i):
                par = i % 3
                kS, ka2, kx1, kidx, kw, kacc = keys(i)
                allA = [("araw", r) for r in range(128)]
                self.op("dve", "tensor_tensor", allA, ["t1"], out=t1[:], in0=araw[:], in1=araw[:], op=ALU.mult)
                self.op("dve", "tensor_scalar", ["t1"], ["t1"], out=t1[:], in0=t1[:], scalar1=0.044715,
                        scalar2=1.0, op0=ALU.mult, op1=ALU.add)
                self.op("dve", "tensor_tensor", ["t1"] + allA, ["t1"], out=t1[:], in0=t1[:], in1=araw[:],
                        op=ALU.mult)
                self.op("act", "activation", ["t1"], ["t2"], out=t2[:], in_=t1[:], func=AF.Tanh, scale=C0)
                self.op("dve", "tensor_scalar", ["t2"], ["t2"], out=t2[:], in0=t2[:], scalar1=1.0,
                        scalar2=0.5, op0=ALU.add, op1=ALU.mult)
                self.op("dve", "tensor_tensor", ["t2"] + allA, ["t2"], out=t2[:], in0=t2[:], in1=araw[:],
                        op=ALU.mult)
                self.op("dve", "tensor_tensor", ["t2", ("gte", par)], [kw], out=wgt[par][:], in0=t2[:],
                        in1=gte[par][:], op=ALU.mult)

            def vstep(i, r):
                par = i % 3
                kS, ka2, kx1, kidx, kw, kacc = keys(i)
                if True:
                    s_ = r % NS
                    g_ = r % 4
                    self.s.add("pool", (lambda e, o=vb[s_], ix=idx[par], r=r: e.indirect_dma_start(
                        out=o[:], out_offset=None, in_=vtab,
                        in_offset=bass.IndirectOffsetOnAxis(ap=ix[:, r:r + 1], axis=0))),
                        [kidx], [("vb", s_)], dma=True, slot=NS + s_)
                    self.op("act", "activation", ["pident", kw], [("dg", g_)], out=dg[g_][:], in_=ident[:],
                            func=AF.Copy, scale=wgt[par][:, r:r + 1])
                    for n in range(2):
                        ns = slice(n * 512, (n + 1) * 512)
                        self.op("pe", "matmul", [("dg", g_), ("vb", s_)], [("accp", i % 2)], accp[i % 2][:, ns],
                                dg[g_][:], vb[s_][:, ns], start=(r == 0), stop=(r == 127))

            def final(i):
                b, tl = tiles[i]
                par = i % 3
                rows = slice(tl * 128, (tl + 1) * 128)
                kS, ka2, kx1, kidx, kw, kacc = keys(i)
                self.op("dve", "tensor_tensor", [("accp", i % 2), "g5"], [kacc], out=acc[par][:],
                        in0=accp[i % 2][:], in1=g5[who_par[i]][:], op=ALU.mult)
                self.op("dve", "tensor_tensor", [kacc, kx1], [kacc], out=acc[par][:], in0=acc[par][:],
                        in1=x1[par][:], op=ALU.add)
                if final_:
                    lrow = slice((tl - NCT) * 128, (tl - NCT + 1) * 128)
                    self.dma("sp", d["y"][b, lrow, :], acc[par][:], [kacc], [("y", b, tl)])
                else:
                    self.dma("sp", d["xs"][b, rows, :], acc[par][:], [kacc], [("xs", b, tl)])

            def body():
                self.dma("sp", iota[:], d["iota256"][:, :], [], ["iota"])
                self.dma("sp", ident[:], d["ident"][:, :], [], ["pident"])
                cur = None
                nwho = 0
                for i, (b, tl) in enumerate(tiles):
                    who = 2 if tl < NCT else b
                    if who != cur:
                        cur = who
                        nwho += 1
                        who_first[i] = (who, (nwho - 1) % 2)
                    who_par[i] = (nwho - 1) % 2
                def maybe_g5(i):
                    if i in who_first:
                        who, gp_ = who_first[i]
                        self.load_mod(g5[gp_], l, who, 5, "g5")
                maybe_g5(0)
                loads(0)
                for _ in topk(0):
                    pass
                for i in range(n_t + 1):
                    gen = None
                    if i + 1 < n_t:
                        maybe_g5(i + 1)
                        loads(i + 1)
                        gen = topk(i + 1)
                    for r in range(128):
                        if i < n_t:
                            ustep(i, r)
                        if i > 0:
                            vstep(i - 1, r)
                        if gen is not None:
                            for _ in range(3):
                                if next(gen, "done") == "done":
                                    gen = None
                                    break
                    if i < n_t:
                        gelu(i)
                    if i > 0:
                        final(i - 1)
                    if gen is not None:
                        for _ in gen:
                            pass
            self.phase(body)


def build_program(cfg):
    from contextlib import ExitStack
    nc = bass.Bass("TRN2", target_bir_lowering=False)
    B = Builder(nc, cfg)
    B.din("xin", [NB, T, D])
    B.din("cT3", [128, 8, 3])
    B.din("w_mod", [DEPTH, D, 6 * D])
    B.din("b_mod", [DEPTH, 6 * D])
    B.din("g_mix", [DEPTH, D])
    B.din("g_ffn", [DEPTH, D])
    B.din("w_out_even", [2, D, D])
    B.din("w_out_odd", [2, D, D])
    B.din("peer_wq", [DEPTH, D, D])
    B.din("kbd", [DEPTH, 128, 8, 256])
    B.din("peer_u", [DEPTH, NEXP, D])
    B.din("peer_v", [DEPTH, NEXP, D])
    B.din("ident", [128, 128])
    B.din("iota256", [128, 256])
    B.din("cos64", [SEQ, 64])
    B.din("sin64", [SEQ, 64])
    B.din("tri4", [128, 4, 128])
    B.din("w_in_even", [2, D, 3104])
    B.din("w_in_odd", [2, D, 1536])
    for nm, shp in (("a_q_gain", [2, 64]), ("a_k_gain", [2, 64]), ("a_lambda", [2, 4, 64]), ("a_subln", [2, 128]),
                    ("b_w_af", [2, 16, 256]), ("b_b_af", [2, 256]), ("b_w_ab", [2, 16, 256]), ("b_b_ab", [2, 256]),
                    ("b_gain", [2, 128]), ("c_q_gain", [2, 64]), ("c_k_gain", [2, 64])):
        B.din(nm, shp)
    B.dscr("QT", [NB, 16, 64, T])
    B.dscr("KT", [NB, 8, 64, T])
    B.dscr("V", [NB, T, 512])
    B.dscr("G", [NB, T, 2048])
    if cfg.get("mx_input"):
        B.din("Mx", [NB, T, D])
    else:
        B.dscr("Mx", [NB, T, D])
    B.dout("y", [NB, SEQ, D])
    B.dscr("xs", [NB, T, D])
    B.dscr("A2", [NB, T, D])
    B.dscr("Sc", [NB, T, 2048])
    B.dscr("modD", [DEPTH, 3, 6 * D])
    with ExitStack() as st:
        B.esem = {e: st.enter_context(nc.semaphore(f"es_{e}")) for e in Sched.ENGS}
        B.dsem = {"sp": [st.enter_context(nc.semaphore(f"ds_sp{i}")) for i in range(8)],
                  "act": [st.enter_context(nc.semaphore(f"ds_act{i}")) for i in range(8)],
                  "pool": [st.enter_context(nc.semaphore(f"ds_pool{i}")) for i in range(16)],
                  "pe": [], "dve": []}
        cfg["program"](B)
    return nc


def full_program(B, layers=range(DEPTH), bs=range(NB), skip_p4=(), skip=()):
    B.phase_mod(layers)
    alltiles = [(b, tl) for b in bs for tl in range(NT)]
    for l in layers:
        last = l == DEPTH - 1
        xsrc = "xin" if l == 0 else "xs"
        B.phase_p1(l, alltiles, xsrc)
        if "attn" not in skip:
            B.phase_attn(l, list(bs), not last)
        if l % 2 == 0 and "gla" not in skip:
            B.phase_gla(l, list(bs))
        tiles = [(b, tl) for (b, tl) in alltiles if not (last and tl < NCT)]
        B.phase_p3(l, tiles, xsrc)
        if l not in skip_p4:
            B.phase_p4(l, tiles, last)


def prep_consts():
    ident = np.eye(128, dtype=np.float32)
    iota = np.tile(np.arange(256, dtype=np.float32)[None, :], (128, 1))
    pos = np.arange(SEQ)
    rc = np.stack([(pos // GRID_W).astype(np.float32), (pos % GRID_W).astype(np.float32)], axis=-1)
    inv = (np.float32(10000.0) ** (-np.arange(16, dtype=np.float32) / np.float32(16))).astype(np.float32)
    ang = (rc[:, :, None] * inv[None, None, :]).astype(np.float32)
    c, s_ = np.cos(ang).astype(np.float32), np.sin(ang).astype(np.float32)
    cos64 = np.stack([c, c], axis=2).reshape(SEQ, 64)
    sin64 = np.stack([-s_, s_], axis=2).reshape(SEQ, 64)
    sp, cc = np.meshgrid(np.arange(128), np.arange(128), indexing="ij")
    tri4 = np.stack([sp <= cc, sp > cc, sp >= cc, sp < cc], axis=1).astype(np.float32)
    return {"ident": ident, "iota256": iota, "cos64": np.ascontiguousarray(cos64),
            "sin64": np.ascontiguousarray(sin64), "tri4": np.ascontiguousarray(tri4)}


def prep_core_inputs(inp, core):
    bs = slice(core * NB, (core + 1) * NB)
    xin = np.concatenate([inp["ctx"][bs], inp["x"][bs]], axis=1).astype(np.float32)
    cv = np.stack([inp["c"][core * NB + 0], inp["c"][core * NB + 1], inp["c_ctx"]], axis=1)
    cT3 = np.ascontiguousarray(cv.reshape(8, 128, 3).transpose(1, 0, 2)).astype(np.float32)
    keys = inp["peer_keys"]
    kbd = np.zeros((DEPTH, 128, 8, 256), np.float32)
    kbd[:, 0:64, :, 0:128] = keys[:, :, 0].transpose(0, 3, 1, 2)
    kbd[:, 64:128, :, 128:256] = keys[:, :, 1].transpose(0, 3, 1, 2)
    m = {"xin": np.ascontiguousarray(xin), "cT3": cT3, "kbd": kbd}
    for k in ("w_mod", "b_mod", "g_mix", "g_ffn", "w_out_even", "w_out_odd", "peer_wq", "peer_u", "peer_v",
              "w_in_even", "w_in_odd", "a_q_gain", "a_k_gain", "a_lambda", "a_subln", "b_w_af", "b_b_af",
              "b_w_ab", "b_b_ab", "b_gain", "c_q_gain", "c_k_gain"):
        m[k] = np.ascontiguousarray(inp[k], dtype=np.float32)
    m.update(prep_consts())
    return m


def phase_p1(self, l, tiles, xsrc):
    from contextlib import ExitStack
    d = self.dram
    even = (l % 2 == 0)
    i2 = l // 2
    if even:
        w_in = d["w_in_even"][i2]; ncols = 3104
        qcols, nqh, kcol0, nkh = 0, 8, 512, 8
        vcol0, vw = 1024, 512
        qg_src, kg_src = d["a_q_gain"], d["a_k_gain"]
    else:
        w_in = d["w_in_odd"][i2]; ncols = 1536
        qcols, nqh, kcol0, nkh = 0, 16, 1024, 4
        vcol0, vw = 1280, 256
        qg_src, kg_src = d["c_q_gain"], d["c_k_gain"]
    nh = nqh + nkh
    nchunk = (ncols + 511) // 512
    with ExitStack() as st:
        win = self.sb(st, "win", [128, 8, ncols])
        ident = self.sb(st, "ident", [128, 128])
        shm = self.sb(st, "shm", [128, D])
        gsm = self.sb(st, "gsm", [128, D])
        gain = self.sb(st, "gain", [128, nh, 64])
        cos = [self.sb(st, f"cos{i}", [128, 64]) for i in range(2)]
        sin = [self.sb(st, f"sin{i}", [128, 64]) for i in range(2)]
        xt = [self.sb(st, f"xt{i}", [128, D]) for i in range(2)]
        a = self.sb(st, "a", [128, D])
        aT = self.sb(st, "aT", [128, D])
        P = self.sb(st, "P", [128, ncols])
        sq = self.sb(st, "sq", [128, nh * 64])
        qs = self.sb(st, "qs", [128, nh * 64])
        ssq = self.sb(st, "ssq", [128, nh])
        ss = self.sb(st, "ss", [128, 2])
        junk = self.sb(st, "junk", [128, D])
        qkT = [self.sb(st, f"qkT{i}", [64, nh, 128]) for i in range(2)]
        tp = self.ps(st, "tp", [128, D])
        pp = [self.ps(st, f"pp{i}", [128, 512]) for i in range(2)]
        hp = [self.ps(st, f"hp{i}", [64, 4, 128]) for i in range(2)]
        if even:
            G = [self.sb(st, f"G{i}", [128, 2048]) for i in range(2)]
            wg = self.sb(st, "wg", [32, 512])
            bg = self.sb(st, "bg", [128, 512])
            pfT = self.sb(st, "pfT", [32, 128])
            gp = self.ps(st, "gp", [128, 512])

        def body():
            for k in range(8):
                self.dma("sp", P[:, :], w_in[k * 128:(k + 1) * 128, :], [], [("P", n) for n in range(nchunk)])
                self.op("dve", "tensor_copy", [("P", n) for n in range(nchunk)], ["win"], out=_r(win[:, k, :]),
                        in_=P[:, :])
            self.dma("act", ident[:], d["ident"][:, :], [], ["ident"])
            for h in range(nh):
                src = (qg_src if h < nqh else kg_src)[i2:i2 + 1, :].to_broadcast([128, 64])
                self.dma("act", gain[:, h, :], src, [], ["gain"])
            self.op("dve", "tensor_scalar", ["gain"], ["gain"], out=gain[:, 0:nqh, :], in0=gain[:, 0:nqh, :],
                    scalar1=0.125, scalar2=None, op0=ALU.mult)
            if even:
                self.op("dve", "memset", [], ["wg"], wg[:], 0.0)
                self.dma("act", wg[0:16, 0:256], d["b_w_af"][i2], ["wg"], ["wg"])
                self.dma("act", wg[16:32, 256:512], d["b_w_ab"][i2], ["wg"], ["wg"])
                self.dma("act", bg[:, 0:256], d["b_b_af"][i2:i2 + 1, :].to_broadcast([128, 256]), [], ["bg"])
                self.dma("act", bg[:, 256:512], d["b_b_ab"][i2:i2 + 1, :].to_broadcast([128, 256]), [], ["bg"])
            cur_who = None
            for i, (b, tl) in enumerate(tiles):
                par = i % 2
                who = 2 if tl < NCT else b
                rows = slice(tl * 128, (tl + 1) * 128)
                latent = tl >= NCT
                if who != cur_who:
                    cur_who = who
                    self.load_mod(shm, l, who, 0, "shm")
                    self.load_mod(gsm, l, who, 1, "gsm")
                kx = ("xt", par)
                self.dma("sp", xt[par][:], d[xsrc][b, rows, :], [("xs", b, tl)], [kx])
                if latent:
                    lr = slice((tl - NCT) * 128, (tl - NCT + 1) * 128)
                    self.dma("act", cos[par][:], d["cos64"][lr, :], [], [("cos", par)])
                    self.dma("act", sin[par][:], d["sin64"][lr, :], [], [("sin", par)])
                self.op("act", "activation", [kx], ["junk", "ss"], out=junk[:], in_=xt[par][:], func=AF.Square,
                        accum_out=ss[:, 0:1])
                self.op("dve", "tensor_scalar", ["ss"], ["ss"], out=ss[:, 0:1], in0=ss[:, 0:1],
                        scalar1=1.0 / D, scalar2=EPS, op0=ALU.mult, op1=ALU.add)
                self.op("act", "activation", ["ss"], ["ss"], out=ss[:, 0:1], in_=ss[:, 0:1], func=AF.Sqrt)
                self.op("dve", "reciprocal", ["ss"], ["ss"], out=ss[:, 0:1], in_=ss[:, 0:1])
                self.op("dve", "scalar_tensor_tensor", [kx, "ss", "gsm"], ["a"], out=a[:], in0=xt[par][:],
                        scalar=ss[:, 0:1], in1=gsm[:], op0=ALU.mult, op1=ALU.mult)
                self.op("dve", "tensor_tensor", ["a", "shm"], ["a"], out=a[:], in0=a[:], in1=shm[:], op=ALU.add)
                for k in range(8):
                    cs = slice(k * 128, (k + 1) * 128)
                    self.op("pe", "transpose", ["a", "ident"], ["tp"], out=tp[:, cs], in_=a[:, cs],
                            identity=ident[:])
                self.op("dve", "tensor_copy", ["tp"], ["aT"], out=_r(aT[:]), in_=tp[:])
                for n in range(nchunk):
                    c0, c1 = n * 512, min(ncols, (n + 1) * 512)
                    pb = n % 2
                    for k in range(8):
                        self.op("pe", "matmul", ["aT", "win"], [("pp", pb)], pp[pb][:, 0:c1 - c0],
                                _r(aT[:, k * 128:(k + 1) * 128]), _r(win[:, k, c0:c1]), start=(k == 0), stop=(k == 7))
                    if n % 2 == 0:
                        self.op("act", "copy", [("pp", pb)], [("P", n)], out=P[:, c0:c1], in_=pp[pb][:, 0:c1 - c0])
                    else:
                        self.op("dve", "tensor_copy", [("pp", pb)], [("P", n)], out=P[:, c0:c1],
                                in_=pp[pb][:, 0:c1 - c0])
                allP = [("P", n) for n in range(nchunk)]
                qk = [(qcols, nqh, 0), (kcol0, nkh, nqh)]
                for (c0, n_, h0) in qk:
                    w_ = n_ * 64
                    src3 = P[:, c0:c0 + w_].rearrange("p (h e) -> p h e", h=n_)
                    sq3 = sq[:, h0 * 64:h0 * 64 + w_].rearrange("p (h e) -> p h e", h=n_)
                    self.op("dve", "tensor_tensor", allP, [("sq", h0)], out=sq3, in0=src3, in1=src3, op=ALU.mult)
                    self.op("dve", "reduce_sum", [("sq", h0)], [("ssq", h0)], out=ssq[:, h0:h0 + n_], in_=sq3,
                            axis=AX.X)
                    self.op("dve", "tensor_scalar", [("ssq", h0)], [("ssq", h0)], out=ssq[:, h0:h0 + n_],
                            in0=ssq[:, h0:h0 + n_], scalar1=1.0 / 64, scalar2=EPS, op0=ALU.mult, op1=ALU.add)
                    self.op("act", "activation", [("ssq", h0)], [("ssq", h0)], out=ssq[:, h0:h0 + n_],
                            in_=ssq[:, h0:h0 + n_], func=AF.Sqrt)
                    self.op("dve", "reciprocal", [("ssq", h0)], [("ssq", h0)], out=ssq[:, h0:h0 + n_],
                            in_=ssq[:, h0:h0 + n_])
                    self.op("dve", "tensor_tensor", allP + [("ssq", h0)], [("sq", h0)], out=sq3, in0=src3,
                            in1=ssq[:, h0:h0 + n_].unsqueeze(2).to_broadcast([128, n_, 64]), op=ALU.mult)
                    self.op("dve", "tensor_tensor", [("sq", h0), "gain"], [("sq", h0)], out=sq3, in0=sq3,
                            in1=gain[:, h0:h0 + n_, :], op=ALU.mult)
                    if latent:
                        x4 = sq[:, h0 * 64:h0 * 64 + w_].rearrange("p (g t f) -> p g t f", t=2, f=16)
                        s4 = qs[:, h0 * 64:h0 * 64 + w_].rearrange("p (g t f) -> p g t f", t=2, f=16)
                        qs3 = qs[:, h0 * 64:h0 * 64 + w_].rearrange("p (h e) -> p h e", h=n_)
                        self.op("dve", "tensor_copy", [("sq", h0)], [("qs", h0)], out=s4[:, :, 0, :], in_=x4[:, :, 1, :])
                        self.op("dve", "tensor_copy", [("sq", h0)], [("qs", h0)], out=s4[:, :, 1, :], in_=x4[:, :, 0, :])
                        self.op("dve", "tensor_tensor", [("sq", h0), ("cos", par)], [("sq", h0)], out=sq3, in0=sq3,
                                in1=cos[par][:, :].unsqueeze(1).to_broadcast([128, n_, 64]), op=ALU.mult)
                        self.op("dve", "tensor_tensor", [("qs", h0), ("sin", par)], [("qs", h0)], out=qs3, in0=qs3,
                                in1=sin[par][:, :].unsqueeze(1).to_broadcast([128, n_, 64]), op=ALU.mult)
                        self.op("dve", "tensor_tensor", [("sq", h0), ("qs", h0)], [("sq", h0)], out=sq3, in0=sq3,
                                in1=qs3, op=ALU.add)
                kq = ("qkT", par)
                for g4 in range(nh // 4):
                    hb = g4 % 2
                    for j in range(4):
                        h = g4 * 4 + j
                        h0 = 0 if h < nqh else nqh
                        self.op("pe", "transpose", [("sq", h0), "ident"], [("hp", hb)], out=hp[hb][:, j, :],
                                in_=sq[:, h * 64:(h + 1) * 64], identity=ident[:])
                    if g4 % 2 == 0:
                        self.op("act", "copy", [("hp", hb)], [kq], out=qkT[par][:, g4 * 4:g4 * 4 + 4, :], in_=hp[hb][:])
                    else:
                        self.op("dve", "tensor_copy", [("hp", hb)], [kq], out=qkT[par][:, g4 * 4:g4 * 4 + 4, :],
                                in_=hp[hb][:])
                self.dma("sp", d["QT"][b, 0:nqh, :, rows].rearrange("h e t -> e h t"), qkT[par][:, 0:nqh, :], [kq],
                         [("QT", b, tl)])
                self.dma("sp", d["KT"][b, 0:nkh, :, rows].rearrange("h e t -> e h t"), qkT[par][:, nqh:nh, :], [kq],
                         [("KT", b, tl)])
                self.dma("act", d["V"][b, rows, 0:vw], P[:, vcol0:vcol0 + vw], allP, [("V", b, tl)])
                if even:
                    Gp = G[par]
                    kG = ("G", par)
                    self.op("dve", "tensor_scalar", allP, [kG], out=Gp[:, 0:256], in0=P[:, 1536:1792],
                            scalar1=0.125, scalar2=None, op0=ALU.mult)
                    self.op("act", "copy", allP, [kG], out=Gp[:, 256:1536], in_=P[:, 1792:3072])
                    self.op("pe", "transpose", allP + ["ident"], [("hp", 0)], out=hp[0][0:32, 0, :],
                            in_=P[:, 3072:3104], identity=ident[:])
                    self.op("act", "copy", [("hp", 0)], ["pfT"], out=pfT[:], in_=hp[0][0:32, 0, :])
                    self.op("pe", "matmul", ["pfT", "wg"], ["gp"], gp[:], pfT[:], wg[:], start=True, stop=True)
                    self.op("dve", "tensor_tensor", ["gp", "bg"], [kG], out=Gp[:, 1536:2048], in0=gp[:], in1=bg[:],
                            op=ALU.add)
                    self.op("act", "activation", [kG], [kG], out=Gp[:, 1536:2048], in_=Gp[:, 1536:2048],
                            func=AF.Exp, scale=-1.0)
                    self.op("act", "activation", [kG], [kG], out=Gp[:, 1536:2048], in_=Gp[:, 1536:2048],
                            func=AF.Ln, bias=1.0)
                    self.op("dve", "tensor_scalar", [kG], [kG], out=Gp[:, 1536:2048], in0=Gp[:, 1536:2048],
                            scalar1=-1.0 / 16.0, scalar2=None, op0=ALU.mult)
                    self.dma("sp", d["G"][b, rows, :], Gp[:], [kG], [("G", b, tl)])
        self.phase(body)


Builder.phase_p1 = phase_p1


def phase_attn(self, l, bs, need_ctx):
    from contextlib import ExitStack
    d = self.dram
    even = (l % 2 == 0)
    i2 = l // 2
    dv = 128 if even else 64
    lam_init = 0.8 - 0.6 * math.exp(-0.3 * l)
    with ExitStack() as st:
        KTs = [[self.sb(st, f"KT{i}{u}", [64, T]) for u in range(2)] for i in range(2)]
        Vp = [self.sb(st, f"Vp{i}", [128, NT, dv + 2], BF16) for i in range(2)]
        QTs = [[self.sb(st, f"QTs{i}{u}", [64, 512]) for u in range(2)] for i in range(2)]
        PT = [self.sb(st, f"PT{i}", [128, 512], BF16) for i in range(3)]
        Kstg = self.sb(st, "Kstg", [64, T])
        Vstg = self.sb(st, "Vstg", [128, NT, dv])
        Qstg = [self.sb(st, f"Qstg{i}", [64, 512]) for i in range(2)]
        rz = self.sb(st, "rz", [128, 2])
        tmp = self.sb(st, "tmp", [128, 128])
        ob = [self.sb(st, f"ob{i}", [128, 128]) for i in range(2)]
        junk = self.sb(st, "junk", [128, 128])
        ss = self.sb(st, "ss", [128, 2])
        Sp = [self.ps(st, f"Sp{i}", [128, 512]) for i in range(2)]
        O = self.ps(st, "O", [128, 6, 512])
        Asb = self.sb(st, "Asb", [128, 4, 128])
        if even:
            lt = self.sb(st, "lt", [128, 4, 64])
            lp = self.sb(st, "lp", [128, 2, 64])
            ls = self.sb(st, "ls", [128, 2])
            nlam = self.sb(st, "nlam", [128, 1])
            sg = self.sb(st, "sg", [128, 128])

        def body():
            for i in range(2):
                self.op("dve", "memset", [], [("Vp", i)], Vp[i][:, :, dv:dv + 2], 1.0)
            if even:
                self.dma("sp", lt[:], d["a_lambda"][i2:i2 + 1, :, :].to_broadcast([128, 4, 64]), [], ["lt"])
                self.dma("sp", sg[:], d["a_subln"][i2:i2 + 1, :].to_broadcast([128, 128]), [], ["sg"])
                self.op("dve", "tensor_scalar", ["sg"], ["sg"], out=sg[:], in0=sg[:], scalar1=1.0 - lam_init,
                        scalar2=None, op0=ALU.mult)
                self.op("dve", "tensor_tensor", ["lt"], ["lp"], out=lp[:, 0, :], in0=lt[:, 0, :], in1=lt[:, 1, :],
                        op=ALU.mult)
                self.op("dve", "tensor_tensor", ["lt"], ["lp"], out=lp[:, 1, :], in0=lt[:, 2, :], in1=lt[:, 3, :],
                        op=ALU.mult)
                self.op("dve", "reduce_sum", ["lp"], ["ls"], out=ls[:], in_=lp[:], axis=AX.X)
                self.op("act", "activation", ["ls"], ["ls"], out=ls[:], in_=ls[:], func=AF.Exp)
                self.op("dve", "tensor_tensor", ["ls"], ["nlam"], out=nlam[:], in0=ls[:, 1:2], in1=ls[:, 0:1],
                        op=ALU.subtract)
                self.op("dve", "tensor_scalar", ["nlam"], ["nlam"], out=nlam[:], in0=nlam[:], scalar1=-lam_init,
                        scalar2=None, op0=ALU.add)
            rnd = [0]
            sidx = [0]
            oslot = [0]
            qidx = 0
            for b in bs:
                for kvh in range(4):
                    vpar = (b * 4 + kvh) % 2
                    kV = ("Vp", vpar)
                    self.dma("sp", Vstg[:],
                             d["V"][b, :, kvh * dv:(kvh + 1) * dv].rearrange("(t p) e -> p t e", p=128),
                             [("V", b, t_) for t_ in range(NT)], ["Vstg"])
                    self.op("dve", "tensor_copy", ["Vstg"], [kV], out=Vp[vpar][:, :, 0:dv], in_=Vstg[:])
                    if even:
                        rounds = [[(2 * kvh, 2 * kvh), (2 * kvh + 1, 2 * kvh + 1)]]
                    else:
                        rounds = [[(kvh * 4 + 0, kvh), (kvh * 4 + 1, kvh)], [(kvh * 4 + 2, kvh), (kvh * 4 + 3, kvh)]]
                    kpar = (b * 4 + kvh) % 2
                    kheads = sorted(set(kh for r_ in rounds for (_, kh) in r_))
                    kmap = {}
                    for u, kh in enumerate(kheads):
                        self.dma("act", Kstg[:], d["KT"][b, kh, :, :], [("KT", b, t_) for t_ in range(NT)], ["Kstg"])
                        self.op("dve", "tensor_copy", ["Kstg"], [("KTs", kpar, u)], out=_r(KTs[kpar][u][:]),
                                in_=Kstg[:])
                        kmap[kh] = u
                    for units in rounds:
                        blocks = [(NCT * 128 + j * 512, 512, list(range(NT))) for j in range(SEQ // 512)]
                        if need_ctx:
                            blocks = [(0, CTX, list(range(NCT)))] + blocks
                        for (t0, nq, kts) in blocks:
                            nqs = nq // 128
                            qpar = qidx % 2
                            qidx += 1
                            for u, (qh, kh) in enumerate(units):
                                self.dma("sp", Qstg[u][:, 0:nq], d["QT"][b, qh, :, t0:t0 + nq],
                                         [("QT", b, t_) for t_ in range(t0 // 128, (t0 + nq) // 128)],
                                         [("Qstg", u)])
                                self.op("dve", "tensor_copy", [("Qstg", u)], [("QTs", qpar, u)],
                                        out=_r(QTs[qpar][u][:, 0:nq]), in_=Qstg[u][:, 0:nq])
                            for u, (qh, kh) in enumerate(units):
                                slots = [(oslot[0] + q_) % 6 for q_ in range(nqs)]
                                oslot[0] += nqs
                                ku = kmap[kh]
                                for ki, kt in enumerate(kts):
                                    sb_ = sidx[0] % 2
                                    sidx[0] += 1
                                    pb_ = sidx[0] % 3
                                    self.op("pe", "matmul", [("KTs", kpar, ku), ("QTs", qpar, u)], [("Sp", sb_)],
                                            Sp[sb_][:, 0:nq], _r(KTs[kpar][ku][:, kt * 128:(kt + 1) * 128]),
                                            _r(QTs[qpar][u][:, 0:nq]), start=True, stop=True)
                                    self.op("act", "activation", [("Sp", sb_)], [("PT", pb_)], out=PT[pb_][:, 0:nq],
                                            in_=Sp[sb_][:, 0:nq], func=AF.Exp)
                                    for qs_ in range(nqs):
                                        slot = slots[qs_]
                                        self.op("pe", "matmul", [("PT", pb_), kV], [("O", slot)],
                                                O[:, slot, 0:dv + 2], PT[pb_][:, qs_ * 128:(qs_ + 1) * 128],
                                                Vp[vpar][:, kt, :], start=(ki == 0), stop=(ki == len(kts) - 1))
                                for qs_ in range(nqs):
                                    slot = slots[qs_]
                                    tl = t0 // 128 + qs_
                                    rows = slice(tl * 128, (tl + 1) * 128)
                                    self.op("dve", "reciprocal", [("O", slot)], ["rz"], out=rz[:, 0:1],
                                            in_=O[:, slot, dv:dv + 1])
                                    if even and u == 0:
                                        self.op("dve", "tensor_scalar", [("O", slot), "rz"], [("Asb", qs_)],
                                                out=Asb[:, qs_, :], in0=O[:, slot, 0:dv], scalar1=rz[:, 0:1],
                                                scalar2=None, op0=ALU.mult)
                                    elif even:
                                        opar = rnd[0] % 2
                                        rnd[0] += 1
                                        ko = ("ob", opar)
                                        self.op("dve", "tensor_scalar", [("O", slot), "rz", "nlam"], ["tmp"], out=tmp[:],
                                                in0=O[:, slot, 0:dv], scalar1=rz[:, 0:1], scalar2=nlam[:, 0:1],
                                                op0=ALU.mult, op1=ALU.mult)
                                        self.op("dve", "tensor_tensor", ["tmp", ("Asb", qs_)], ["tmp"], out=tmp[:],
                                                in0=tmp[:], in1=Asb[:, qs_, :], op=ALU.add)
                                        self.op("act", "activation", ["tmp"], ["junk", "ss"], out=junk[:], in_=tmp[:],
                                                func=AF.Square, accum_out=ss[:, 0:1])
                                        self.op("dve", "tensor_scalar", ["ss"], ["ss"], out=ss[:, 0:1], in0=ss[:, 0:1],
                                                scalar1=1.0 / 128, scalar2=EPS, op0=ALU.mult, op1=ALU.add)
                                        self.op("act", "activation", ["ss"], ["ss"], out=ss[:, 0:1], in_=ss[:, 0:1],
                                                func=AF.Sqrt)
                                        self.op("dve", "reciprocal", ["ss"], ["ss"], out=ss[:, 0:1], in_=ss[:, 0:1])
                                        self.op("dve", "scalar_tensor_tensor", ["tmp", "ss", "sg"], [ko],
                                                out=ob[opar][:], in0=tmp[:], scalar=ss[:, 0:1], in1=sg[:],
                                                op0=ALU.mult, op1=ALU.mult)
                                        self.dma("sp", d["Mx"][b, rows, kvh * 128:(kvh + 1) * 128], ob[opar][:], [ko],
                                                 [("Mx", b, tl, kvh)])
                                    else:
                                        opar = rnd[0] % 2
                                        rnd[0] += 1
                                        ko = ("ob", opar)
                                        self.op("dve", "tensor_scalar", [("O", slot), "rz"], [ko],
                                                out=ob[opar][:, 0:dv], in0=O[:, slot, 0:dv], scalar1=rz[:, 0:1],
                                                scalar2=None, op0=ALU.mult)
                                        self.dma("sp", d["Mx"][b, rows, qh * 64:(qh + 1) * 64], ob[opar][:, 0:dv],
                                                 [ko], [("Mx", b, tl, qh)])
        self.phase(body)


Builder.phase_attn = phase_attn


def phase_gla(self, l, bs):
    from contextlib import ExitStack
    d = self.dram
    i2 = l // 2
    with ExitStack() as st:
        tri = self.sb(st, "tri", [128, 4, 128])
        ident = self.sb(st, "ident", [128, 128])
        gg = self.sb(st, "gg", [128, 128])
        Gt = [self.sb(st, f"Gt{i}", [128, 2048]) for i in range(2)]
        Of = self.sb(st, "Of", [128, NT, 512])
        ek = self.sb(st, "ek", [128, 256])
        ke = self.sb(st, "ke", [128, 256])
        eT = self.sb(st, "eT", [64, 4, 128])
        eiT = self.sb(st, "eiT", [64, 4, 128])
        qdT = self.sb(st, "qdT", [64, 4, 128])
        kiT = self.sb(st, "kiT", [64, 4, 128])
        Am = [self.sb(st, f"Am{i}", [128, 128]) for i in range(2)]
        H = [self.sb(st, f"H{i}", [64, 4, 128]) for i in range(2)]
        osum = self.sb(st, "osum", [128, 128])
        sr = self.sb(st, "sr", [128, 128])
        gout = [self.sb(st, f"gout{i}", [128, 512]) for i in range(2)]
        junk = self.sb(st, "junk", [128, 128])
        ss = self.sb(st, "ss", [128, 2])
        cr = self.ps(st, "cr", [128, 256])
        ct = self.ps(st, "ct", [64, 4, 128])
        gT = self.ps(st, "gT", [64, 8, 128])
        At = self.ps(st, "At", [128, 2, 128])
        op_ = self.ps(st, "op", [128, 2, 128])
        dS = self.ps(st, "dS", [64, 2, 128])

        def body():
            self.dma("sp", tri[:], d["tri4"][:, :, :], [], ["tri"])
            self.dma("sp", ident[:], d["ident"][:, :], [], ["ident"])
            self.dma("sp", gg[:], d["b_gain"][i2:i2 + 1, :].to_broadcast([128, 128]), [], ["gg"])
            cnt = 0
            hcnt = 0
            for b in bs:
                for dr in range(2):
                    order = list(range(NT)) if dr == 0 else [1, 0] + list(range(NT - 1, NCT - 1, -1))
                    lac = 1536 + 256 * dr
                    incl, strict = tri[:, 2 * dr, :], tri[:, 2 * dr + 1, :]
                    hp_ = cnt % 2
                    self.op("dve", "memset", [], [("H", hp_)], H[hp_][:], 0.0)
                    for tl in order:
                        par = cnt % 2
                        cnt += 1
                        Hc, Hn = H[(cnt - 1) % 2], H[cnt % 2]
                        kHc, kHn = ("H", (cnt - 1) % 2), ("H", cnt % 2)
                        rows = slice(tl * 128, (tl + 1) * 128)
                        G_ = Gt[par]
                        kG = ("Gt", par)
                        self.dma("sp", G_[:], d["G"][b, rows, :], [("G", b, tl)], [kG])
                        la = G_[:, lac:lac + 256]
                        self.op("pe", "matmul", [kG, "tri"], ["cr"], cr[:], strict, la, start=True, stop=True)
                        self.op("act", "activation", ["cr"], ["ek"], out=ek[:], in_=cr[:], func=AF.Exp)
                        self.op("dve", "tensor_tensor", ["ek", kG], ["ke"], out=ke[:], in0=ek[:], in1=G_[:, 256:512],
                                op=ALU.mult)
                        for h in range(4):
                            self.op("pe", "matmul", [kG, "tri"], ["ct"], ct[:, h, :], G_[:, lac + h * 64:lac + (h + 1) * 64],
                                    incl, start=True, stop=True)
                        self.op("act", "activation", ["ct"], ["eT"], out=eT[:], in_=ct[:], func=AF.Exp)
                        self.op("act", "activation", ["ct"], ["eiT"], out=eiT[:], in_=ct[:], func=AF.Exp, scale=-1.0)
                        for h in range(4):
                            self.op("pe", "transpose", [kG, "ident"], ["gT"], out=gT[:, h, :],
                                    in_=G_[:, h * 64:(h + 1) * 64], identity=ident[:])
                            self.op("pe", "transpose", [kG, "ident"], ["gT"], out=gT[:, 4 + h, :],
                                    in_=G_[:, 256 + h * 64:256 + (h + 1) * 64], identity=ident[:])
                        self.op("dve", "tensor_tensor", ["gT", "eT"], ["qdT"], out=qdT[:], in0=gT[:, 0:4, :], in1=eT[:],
                                op=ALU.mult)
                        self.op("dve", "tensor_tensor", ["gT", "eiT"], ["kiT"], out=kiT[:], in0=gT[:, 4:8, :],
                                in1=eiT[:], op=ALU.mult)
                        dcol = 127 if dr == 0 else 0
                        kgo = ("gout", par)
                        for h in range(4):
                            ap_ = hcnt % 2
                            hcnt += 1
                            gv = G_[:, 512 + h * 128:512 + (h + 1) * 128]
                            self.op("pe", "matmul", ["kiT", "qdT"], [("At", ap_)], At[:, ap_, :], kiT[:, h, :],
                                    qdT[:, h, :], start=True, stop=True)
                            self.op("dve", "tensor_tensor", [("At", ap_), "tri"], [("Am", ap_)], out=Am[ap_][:],
                                    in0=At[:, ap_, :], in1=incl, op=ALU.mult)
                            self.op("pe", "matmul", [("Am", ap_), kG], [("op", ap_)], op_[:, ap_, :], Am[ap_][:], gv,
                                    start=True, stop=False)
                            self.op("pe", "matmul", ["qdT", kHc], [("op", ap_)], op_[:, ap_, :], qdT[:, h, :],
                                    Hc[:, h, :], start=False, stop=True)
                            self.op("pe", "matmul", ["ke", kG], [("dS", ap_)], dS[:, ap_, :], ke[:, h * 64:(h + 1) * 64],
                                    gv, start=True, stop=True)
                            self.op("dve", "scalar_tensor_tensor", [kHc, "eT", ("dS", ap_)], [kHn], out=Hn[:, h, :],
                                    in0=Hc[:, h, :], scalar=eT[:, h, dcol:dcol + 1], in1=dS[:, ap_, :],
                                    op0=ALU.mult, op1=ALU.add)
                            hs = slice(h * 128, (h + 1) * 128)
                            if dr == 0:
                                self.op("act", "copy", [("op", ap_)], [("Of", tl)], out=Of[:, tl, hs],
                                        in_=op_[:, ap_, :])
                            else:
                                self.op("dve", "tensor_tensor", [("op", ap_), ("Of", tl)], ["osum"], out=osum[:],
                                        in0=op_[:, ap_, :], in1=Of[:, tl, hs], op=ALU.add)
                                self.op("act", "activation", ["osum"], ["junk", "ss"], out=junk[:], in_=osum[:],
                                        func=AF.Square, accum_out=ss[:, 0:1])
                                self.op("dve", "tensor_scalar", ["ss"], ["ss"], out=ss[:, 0:1], in0=ss[:, 0:1],
                                        scalar1=1.0 / 128, scalar2=EPS, op0=ALU.mult, op1=ALU.add)
                                self.op("act", "activation", ["ss"], ["ss"], out=ss[:, 0:1], in_=ss[:, 0:1],
                                        func=AF.Sqrt)
                                self.op("dve", "reciprocal", ["ss"], ["ss"], out=ss[:, 0:1], in_=ss[:, 0:1])
                                self.op("dve", "scalar_tensor_tensor", ["osum", "ss", "gg"], ["osum"], out=osum[:],
                                        in0=osum[:], scalar=ss[:, 0:1], in1=gg[:], op0=ALU.mult, op1=ALU.mult)
                                self.op("act", "activation", [kG], ["sr"], out=sr[:],
                                        in_=G_[:, 1024 + h * 128:1024 + (h + 1) * 128], func=AF.Silu)
                                self.op("dve", "tensor_tensor", ["osum", "sr"], [kgo], out=gout[par][:, hs],
                                        in0=osum[:], in1=sr[:], op=ALU.mult)
                        if dr == 1:
                            self.dma("sp", d["Mx"][b, rows, 512:1024], gout[par][:], [kgo], [("Mxg", b, tl)])
        self.phase(body)


Builder.phase_gla = phase_gla


N_CORES = 8


def kernel(**inputs):
    inp = {k: np.asarray(v) for k, v in inputs.items()}
    cfg = {"program": full_program}
    nc = build_program(cfg)
    in_maps = [prep_core_inputs(inp, c) for c in range(N_CORES)]
    res = run_bass_kernel_spmd(nc, in_maps, core_ids=list(range(N_CORES)))
    out = np.concatenate([np.asarray(r["y"]) for r in res.results], axis=0)
    return out.astype(np.float32)
```

```python
import math
import numpy as np
import concourse.bass as bass
import concourse.mybir as mybir
from concourse.bass_utils import run_bass_kernel_spmd

F32 = mybir.dt.float32
U32 = mybir.dt.uint32
F32R = mybir.dt.float32r
BF16 = mybir.dt.bfloat16


def _r(ap):
    return ap.bitcast(F32R)
AF = mybir.ActivationFunctionType
ALU = mybir.AluOpType
AX = mybir.AxisListType

D = 1024
DEPTH = 4
NB = 2
CTX = 256
SEQ = 2048
T = CTX + SEQ
NT = T // 128
NCT = CTX // 128
HD = 64
GRID_W = 64
EPS = 1e-6
NEXP = 16384
NEG = -1.0e30


class _Op:
    __slots__ = ("eng", "fn", "reads", "writes", "dma", "idx", "stream", "spos",
                 "waits", "signal", "sigval", "slot")


class Sched:
    ENGS = ("sp", "act", "pool", "pe", "dve")

    def __init__(self, nc, esem, dsem, dbase=None):
        self.nc = nc
        self.esem = esem
        self.dsem = dsem
        self.dbase = dbase if dbase is not None else {}
        self.ops = []

    def add(self, eng, fn, reads=(), writes=(), dma=False, slot=None):
        op = _Op()
        op.eng, op.fn, op.reads, op.writes, op.dma = eng, fn, tuple(reads), tuple(writes), dma
        op.slot = slot
        op.idx = len(self.ops)
        op.signal = False
        op.waits = ()
        self.ops.append(op)
        return op

    def _analyse(self):
        ops = self.ops
        last_w, readers = {}, {}
        pos = {e: 0 for e in self.ENGS}
        dcnt = {e: self.dbase.get(e, 0) for e in self.ENGS}
        slot_last = {}
        slot_last = {}
        clock = {e: {} for e in self.ENGS}
        for op in ops:
            deps = set()
            for r in op.reads:
                w = last_w.get(r)
                if w is not None:
                    deps.add(w)
            for k in op.writes:
                w = last_w.get(k)
                if w is not None:
                    deps.add(w)
                deps.update(readers.get(k, ()))
            for k in op.writes:
                last_w[k] = op.idx
                readers[k] = []
            for r in op.reads:
                readers.setdefault(r, []).append(op.idx)
            deps.discard(op.idx)
            if op.dma and op.slot is not None:
                key = (op.eng, op.slot)
                j = self.dbase.get(key, 0)
                self.dbase[key] = j + 1
                op.stream = key
                op.spos = j
                prev = slot_last.get(op.stream)
                if prev is not None:
                    deps.add(prev)
                slot_last[op.stream] = op.idx
            elif op.dma:
                K = len(self.dsem[op.eng])
                j = dcnt[op.eng]
                dcnt[op.eng] += 1
                op.stream = (op.eng, j % K)
                op.spos = j // K
                prev = slot_last.get(op.stream)
                if prev is not None:
                    deps.add(prev)
                slot_last[op.stream] = op.idx
            else:
                op.stream = op.eng
                op.spos = pos[op.eng]
            pos[op.eng] += 1
            need = {}
            implied = {}
            for d in deps:
                P = ops[d]
                if (not P.dma) and (not op.dma) and P.eng == "pe" and op.eng == "pe":
                    continue
                if need.get(P.stream, (-1, None))[0] < P.spos:
                    need[P.stream] = (P.spos, d)
                for q in P.waits:
                    Q = ops[q]
                    if implied.get(Q.stream, -1) < Q.spos:
                        implied[Q.stream] = Q.spos
            ck = clock[op.eng]
            waits = []
            for stream, (sp_, d) in need.items():
                if ck.get(stream, -1) >= sp_:
                    continue
                if implied.get(stream, -1) >= sp_:
                    continue
                waits.append(d)
                ops[d].signal = True
            for stream, (sp_, d) in need.items():
                if ck.get(stream, -1) < sp_:
                    ck[stream] = sp_
            for stream, sp_ in implied.items():
                if ck.get(stream, -1) < sp_:
                    ck[stream] = sp_
            op.waits = tuple(sorted(waits))
        tails = {}
        for op in ops:
            tails[op.stream] = op.idx
        self.fence = []
        for stream, d in tails.items():
            ops[d].signal = True
            self.fence.append(d)
        cnt = {e: 0 for e in self.ENGS}
        for op in ops:
            if op.dma:
                op.signal = True
                op.sigval = 16 * (op.spos + 1)
            elif op.signal:
                cnt[op.eng] += 1
                op.sigval = cnt[op.eng]
        for e in list(self.dbase):
            if not isinstance(e, tuple):
                self.dbase[e] = dcnt[e]

    def _sem_of(self, P):
        if P.dma:
            return self.dsem[P.stream[0]][P.stream[1]]
        return self.esem[P.eng]

    def emit(self, block):
        self._analyse()
        ops = self.ops
        sched = self

        def run(eng_name):
            def body(e):
                for op in ops:
                    if op.eng != eng_name:
                        continue
                    for d in op.waits:
                        P = ops[d]
                        e.wait_ge(sched._sem_of(P), P.sigval)
                    ins = op.fn(e)
                    if op.dma:
                        ins.then_inc(sched.dsem[op.stream[0]][op.stream[1]], 16)
                    elif op.signal:
                        ins.then_inc(sched.esem[op.eng], 1)
                if eng_name == "sp":
                    for d in sched.fence:
                        P = ops[d]
                        e.wait_ge(sched._sem_of(P), P.sigval)
            return body

        block.sync(run("sp"))
        block.scalar(run("act"))
        block.gpsimd(run("pool"))
        block.tensor(run("pe"))
        block.vector(run("dve"))
        self.ops = []


class Builder:
    def __init__(self, nc, cfg):
        self.nc = nc
        self.cfg = cfg
        self.dram = {}

    def din(self, name, shape, dt=F32):
        t = self.nc.dram_tensor(name, list(shape), dt, kind="ExternalInput").ap()
        self.dram[name] = t
        return t

    def dout(self, name, shape, dt=F32):
        t = self.nc.dram_tensor(name, list(shape), dt, kind="ExternalOutput").ap()
        self.dram[name] = t
        return t

    def dscr(self, name, shape, dt=F32):
        kind = "ExternalOutput" if name in self.cfg.get("dump", ()) else "Internal"
        t = self.nc.dram_tensor(name, list(shape), dt, kind=kind).ap()
        self.dram[name] = t
        return t

    def phase(self, fn):
        nc = self.nc
        with nc.Block(no_gpsimd_drain=True) as blk:
            sems = list(self.esem.values()) + [s for k_, v in self.dsem.items() if k_ != "pool" for s in v]

            def clr(e):
                for s in sems:
                    e.sem_clear(s)
            blk.sync(clr)
        with nc.Block(no_gpsimd_drain=True) as blk:
            if not hasattr(self, "dbase"):
                self.dbase = {"pool": 0}
            self.s = Sched(nc, self.esem, self.dsem, self.dbase)
            fn()
            self.s.emit(blk)

    def add(self, eng, fn, r=(), w=(), dma=False):
        return self.s.add(eng, fn, r, w, dma)

    def load(self, out, in_, r, w, eng="sp"):
        return self.s.add(eng, lambda e: e.dma_start(out=out, in_=in_), r, w, dma=True)

    def op(self, eng, name, r, w, *a, **k):
        return self.s.add(eng, lambda e: getattr(e, name)(*a, **k), r, w)

    def dma(self, eng, out, in_, r, w):
        return self.s.add(eng, lambda e: e.dma_start(out=out, in_=in_), r, w, dma=True)

    def _uname(self, name):
        self._uid = getattr(self, "_uid", 0) + 1
        return f"{name}_{self._uid}"

    def sb(self, st, name, shape, dt=F32):
        return st.enter_context(self.nc.sbuf_tensor(self._uname("s_" + name), list(shape), dt))

    def ps(self, st, name, shape, dt=F32):
        return st.enter_context(self.nc.psum_tensor(self._uname("p_" + name), list(shape), dt))

    def phase_mod(self, layers):
        from contextlib import ExitStack
        d = self.dram
        with ExitStack() as st:
            cT = self.sb(st, "cT", [128, 8, 3])
            bm = self.sb(st, "bm", [3, 6 * D])
            gm = self.sb(st, "gm", [3, D])
            gf = self.sb(st, "gf", [3, D])
            msb = self.sb(st, "msb", [3, 6 * D])
            wb = [self.sb(st, f"wmb{i}", [128, 8, 512]) for i in range(2)]
            mp = [self.ps(st, f"mp{i}", [3, 512]) for i in range(2)]

            def body():
                self.dma("sp", cT[:], d["cT3"][:, :, :], [], ["cT"])
                self.op("act", "activation", ["cT"], ["cT"], out=cT[:], in_=cT[:], func=AF.Silu)
                for l in layers:
                    self.dma("sp", bm[:], d["b_mod"][l:l + 1, :].to_broadcast([3, 6 * D]), [], ["bm"])
                    self.dma("sp", gm[:], d["g_mix"][l:l + 1, :].to_broadcast([3, D]), [], ["gm"])
                    self.dma("sp", gf[:], d["g_ffn"][l:l + 1, :].to_broadcast([3, D]), [], ["gf"])
                    for n in range(12):
                        w_ = wb[n % 2]
                        src = d["w_mod"][l, :, n * 512:(n + 1) * 512].rearrange("(k p) n -> p k n", p=128)
                        self.dma("sp" if n % 2 == 0 else "act", w_[:], src, [], [("wmb", n % 2)])
                        for k in range(8):
                            self.op("pe", "matmul", ["cT", ("wmb", n % 2)], [("mp", n % 2)],
                                    mp[n % 2][:, :], cT[:, k, :], w_[:, k, :], start=(k == 0), stop=(k == 7))
                        self.op("dve", "tensor_tensor", [("mp", n % 2), "bm"], [("msb", n)],
                                out=msb[:, n * 512:(n + 1) * 512], in0=mp[n % 2][:, :],
                                in1=bm[:, n * 512:(n + 1) * 512], op=ALU.add)
                    for (c0, gt, gk) in ((1, gm, "gm"), (4, gf, "gf")):
                        ks = [("msb", 2 * c0), ("msb", 2 * c0 + 1)]
                        sl = msb[:, c0 * D:(c0 + 1) * D]
                        self.op("dve", "tensor_scalar", ks, ks, out=sl, in0=sl, scalar1=1.0, scalar2=None,
                                op0=ALU.add)
                        self.op("dve", "tensor_tensor", ks + [gk], ks, out=sl, in0=sl, in1=gt[:], op=ALU.mult)
                    self.dma("sp", d["modD"][l, :, :], msb[:], [("msb", n) for n in range(12)], [("modD", l)])
            self.phase(body)

    def load_mod(self, dst, l, who, j, key, eng="sp"):
        src = self.dram["modD"][l, who:who + 1, j * D:(j + 1) * D].to_broadcast([128, D])
        self.dma(eng, dst[:], src, [], [key])

    def phase_p3(self, l, tiles, xsrc="xs"):
        from contextlib import ExitStack
        d = self.dram
        w_out = d["w_out_even"][l // 2] if l % 2 == 0 else d["w_out_odd"][l // 2]
        with ExitStack() as st:
            wo = self.sb(st, "wo", [128, 8, D])
            wq = self.sb(st, "wq", [128, 8, D])
            kbd = self.sb(st, "kbd", [128, 8, 256])
            ident = self.sb(st, "ident", [128, 128])
            g2 = self.sb(st, "g2", [128, D])
            shf = self.sb(st, "shf", [128, D])
            gsf = self.sb(st, "gsf", [128, D])
            Mt = [self.sb(st, f"Mt{i}", [128, D]) for i in range(2)]
            xt = [self.sb(st, f"xt{i}", [128, D]) for i in range(2)]
            x1 = [self.sb(st, f"x1{i}", [128, D]) for i in range(2)]
            a2 = [self.sb(st, f"a2{i}", [128, D]) for i in range(2)]
            Ssb = [self.sb(st, f"Ssb{i}", [128, 2048]) for i in range(2)]
            MT = self.sb(st, "MT", [128, D])
            a2T = self.sb(st, "a2T", [128, D])
            q = self.sb(st, "q", [128, D])
            qT = self.sb(st, "qT", [128, D])
            junk = self.sb(st, "junk", [128, D])
            ss = self.sb(st, "ss", [128, 2])
            tp = self.ps(st, "tp", [128, D])
            yp = self.ps(st, "yp", [128, D])
            spm = self.ps(st, "spm", [128, 2048])

            def body():
                for k in range(8):
                    self.dma("sp", junk[:], w_out[k * 128:(k + 1) * 128, :], [], ["junk"])
                    self.op("dve", "tensor_copy", ["junk"], ["wo"], out=_r(wo[:, k, :]), in_=junk[:])
                for k in range(8):
                    self.dma("act", q[:], d["peer_wq"][l, k * 128:(k + 1) * 128, :], [], ["q"])
                    self.op("dve", "tensor_copy", ["q"], ["wq"], out=_r(wq[:, k, :]), in_=q[:])
                self.dma("sp", Ssb[0][:, :].rearrange("p (h n) -> p h n", h=8), d["kbd"][l], [], [("Ssb", 0)])
                self.op("dve", "tensor_copy", [("Ssb", 0)], ["kbd"], out=_r(kbd[:]),
                        in_=Ssb[0][:, :].rearrange("p (h n) -> p h n", h=8))
                self.dma("sp", ident[:], d["ident"][:, :], [], ["ident"])
                cur_who = None
                for i, (b, tl) in enumerate(tiles):
                    par = i % 2
                    who = 2 if tl < NCT else b
                    rows = slice(tl * 128, (tl + 1) * 128)
                    if who != cur_who:
                        cur_who = who
                        self.load_mod(g2, l, who, 2, "g2")
                        self.load_mod(shf, l, who, 3, "shf")
                        self.load_mod(gsf, l, who, 4, "gsf")
                    kM, kx, kx1, ka2, kS = ("Mt", par), ("xt", par), ("x1", par), ("a2", par), ("Ssb", par)
                    self.dma("sp", Mt[par][:], d["Mx"][b, rows, :], [("Mx", b, tl)], [kM])
                    self.dma("act", xt[par][:], d[xsrc][b, rows, :], [("xs", b, tl)], [kx])
                    for k in range(8):
                        cs = slice(k * 128, (k + 1) * 128)
                        self.op("pe", "transpose", [kM, "ident"], ["tp"], out=tp[:, cs], in_=Mt[par][:, cs],
                                identity=ident[:])
                    self.op("dve", "tensor_copy", ["tp"], ["MT"], out=_r(MT[:]), in_=tp[:])
                    for n in range(2):
                        ns = slice(n * 512, (n + 1) * 512)
                        for k in range(8):
                            self.op("pe", "matmul", ["MT", "wo"], ["yp"], yp[:, ns], _r(MT[:, k * 128:(k + 1) * 128]),
                                    _r(wo[:, k, ns]), start=(k == 0), stop=(k == 7))
                    self.op("dve", "tensor_tensor", ["yp", "g2"], [kx1], out=x1[par][:], in0=yp[:], in1=g2[:],
                            op=ALU.mult)
                    self.op("dve", "tensor_tensor", [kx1, kx], [kx1], out=x1[par][:], in0=x1[par][:],
                            in1=xt[par][:], op=ALU.add)
                    self.dma("sp", d["xs"][b, rows, :], x1[par][:], [kx1], [("xs", b, tl)])
                    self.op("act", "activation", [kx1], ["junk", "ss"], out=junk[:], in_=x1[par][:], func=AF.Square,
                            accum_out=ss[:, 0:1])
                    self.op("dve", "tensor_scalar", ["ss"], ["ss"], out=ss[:, 0:1], in0=ss[:, 0:1],
                            scalar1=1.0 / D, scalar2=EPS, op0=ALU.mult, op1=ALU.add)
                    self.op("act", "activation", ["ss"], ["ss"], out=ss[:, 0:1], in_=ss[:, 0:1], func=AF.Sqrt)
                    self.op("dve", "reciprocal", ["ss"], ["ss"], out=ss[:, 0:1], in_=ss[:, 0:1])
                    self.op("dve", "scalar_tensor_tensor", [kx1, "ss", "gsf"], [ka2], out=a2[par][:],
                            in0=x1[par][:], scalar=ss[:, 0:1], in1=gsf[:], op0=ALU.mult, op1=ALU.mult)
                    self.op("dve", "tensor_tensor", [ka2, "shf"], [ka2], out=a2[par][:], in0=a2[par][:],
                            in1=shf[:], op=ALU.add)
                    self.dma("sp", d["A2"][b, rows, :], a2[par][:], [ka2], [("A2", b, tl)])
                    for k in range(8):
                        cs = slice(k * 128, (k + 1) * 128)
                        self.op("pe", "transpose", [ka2, "ident"], ["tp"], out=tp[:, cs], in_=a2[par][:, cs],
                                identity=ident[:])
                    self.op("dve", "tensor_copy", ["tp"], ["a2T"], out=_r(a2T[:]), in_=tp[:])
                    for n in range(2):
                        ns = slice(n * 512, (n + 1) * 512)
                        for k in range(8):
                            self.op("pe", "matmul", ["a2T", "wq"], ["yp"], yp[:, ns], _r(a2T[:, k * 128:(k + 1) * 128]),
                                    _r(wq[:, k, ns]), start=(k == 0), stop=(k == 7))
                    self.op("act", "copy", ["yp"], ["q"], out=q[:], in_=yp[:])
                    for k in range(8):
                        cs = slice(k * 128, (k + 1) * 128)
                        self.op("pe", "transpose", ["q", "ident"], ["tp"], out=tp[:, cs], in_=q[:, cs],
                                identity=ident[:])
                    self.op("dve", "tensor_copy", ["tp"], ["qT"], out=_r(qT[:]), in_=tp[:])
                    for h in range(8):
                        self.op("pe", "matmul", ["qT", "kbd"], ["spm"], spm[:, h * 256:(h + 1) * 256],
                                _r(qT[:, h * 128:(h + 1) * 128]), _r(kbd[:, h, :]), start=True, stop=True)
                    self.op("act", "copy", ["spm"], [kS], out=Ssb[par][:, 0:1024], in_=spm[:, 0:1024])
                    self.op("dve", "tensor_copy", ["spm"], [kS], out=Ssb[par][:, 1024:2048], in_=spm[:, 1024:2048])
                    self.dma("sp", d["Sc"][b, rows, :], Ssb[par][:], [kS], [("Sc", b, tl)])
            self.phase(body)

    def phase_p4(self, l, tiles, final):
        from contextlib import ExitStack
        d = self.dram
        NS = 6
        C0 = 0.7978845608028654
        with ExitStack() as st:
            S = [self.sb(st, f"S{i}", [128, 2048]) for i in range(3)]
            a2 = [self.sb(st, f"pa2{i}", [128, D]) for i in range(3)]
            x1 = [self.sb(st, f"px1{i}", [128, D]) for i in range(3)]
            idx = [self.sb(st, f"idx{i}", [128, 128], U32) for i in range(3)]
            wgt = [self.sb(st, f"wgt{i}", [128, 128]) for i in range(3)]
            acc = [self.sb(st, f"acc{i}", [128, D]) for i in range(3)]
            S2 = self.sb(st, "S2", [128, 2048])
            V16 = self.sb(st, "V16", [128, 256])
            I16 = self.sb(st, "I16", [128, 256], U32)
            I16f = self.sb(st, "I16f", [128, 256])
            cand = self.sb(st, "cand", [128, 2048])
            cand2 = self.sb(st, "cand2", [128, 2048])
            cidx = self.sb(st, "cidx", [128, 2048])
            best = self.sb(st, "best", [128, 128])
            J = self.sb(st, "J", [128, 128], U32)
            Jf = self.sb(st, "Jf", [128, 128])
            idxf = self.sb(st, "idxf", [128, 128])
            ex = self.sb(st, "ex", [128, 128])
            Z = self.sb(st, "Z", [128, 8])
            gte = [self.sb(st, f"gte{i}", [128, 128]) for i in range(3)]
            who_first, who_par = {}, {}
            final_ = final
            araw = self.sb(st, "araw", [128, 128])
            t1 = self.sb(st, "t1", [128, 128])
            t2 = self.sb(st, "t2", [128, 128])
            iota = self.sb(st, "iota", [128, 256])
            junk = self.ps(st, "pjunk", [128, D])
            a2p = self.ps(st, "a2p", [128, D])
            g5 = [self.sb(st, f"g5{i}", [128, D]) for i in range(2)]
            ub = [self.sb(st, f"ub{i}", [128, D]) for i in range(NS)]
            vb = [self.sb(st, f"vb{i}", [128, D]) for i in range(NS)]
            utab = d["peer_u"].rearrange("l e d -> (l e) d")
            vtab = d["peer_v"].rearrange("l e d -> (l e) d")

            def v3(t, a, b_):
                return t[:, :].rearrange("p (a b) -> p a b", a=a)

            ident = self.sb(st, "pident", [128, 128])
            dg = [self.sb(st, f"dg{i}", [128, 128]) for i in range(4)]
            accp = [self.ps(st, f"accp{i}", [128, D]) for i in range(2)]
            n_t = len(tiles)

            def keys(i):
                par = i % 3
                return ("S", par), ("pa2", par), ("px1", par), ("idx", par), ("wgt", par), ("acc", par)

            def loads(i):
                b, tl = tiles[i]
                par = i % 3
                rows = slice(tl * 128, (tl + 1) * 128)
                kS, ka2, kx1, kidx, kw, kacc = keys(i)
                self.dma("sp", S[par][:], d["Sc"][b, rows, :], [("Sc", b, tl)], [kS])
                self.dma("act", a2[par][:], d["A2"][b, rows, :], [("A2", b, tl)], [ka2])
                self.dma("act", x1[par][:], d["xs"][b, rows, :], [("xs", b, tl)], [kx1])

            def topk(i):
                par = i % 3
                kS, ka2, kx1, kidx, kw, kacc = keys(i)
                Sp = S[par]
                for g in range(16):
                    gs = slice(g * 128, (g + 1) * 128)
                    lo = slice(g * 16, g * 16 + 8)
                    hi = slice(g * 16 + 8, g * 16 + 16)
                    yield self.op("dve", "max", [kS], [("V16", g)], out=V16[:, lo], in_=Sp[:, gs])
                    yield self.op("dve", "max_index", [kS, ("V16", g)], [("I16", g)], out=I16[:, lo],
                            in_max=V16[:, lo], in_values=Sp[:, gs])
                    yield self.op("dve", "match_replace", [kS, ("V16", g)], [("S2", g)], out=S2[:, gs],
                            in_to_replace=V16[:, lo], in_values=Sp[:, gs], imm_value=NEG)
                    yield self.op("dve", "max", [("S2", g)], [("V16", g)], out=V16[:, hi], in_=S2[:, gs])
                    yield self.op("dve", "max_index", [("S2", g), ("V16", g)], [("I16", g)], out=I16[:, hi],
                            in_max=V16[:, hi], in_values=S2[:, gs])
                allV = [("V16", g) for g in range(16)]
                allI = [("I16", g) for g in range(16)]
                yield self.op("dve", "tensor_copy", allI, ["I16f"], out=I16f[:], in_=I16[:])
                I4 = I16f[:, :].rearrange("p (h t k) -> p h t k", h=8, t=2)
                V4 = V16[:, :].rearrange("p (h t k) -> p h t k", h=8, t=2)
                yield self.op("dve", "tensor_scalar", ["I16f"], ["I16f"], out=I4[:, :, 0, :], in0=I4[:, :, 0, :],
                        scalar1=128.0, scalar2=None, op0=ALU.mult)
                c4 = cand[:, :].rearrange("p (h a c) -> p h a c", h=8, a=16)
                x4 = cidx[:, :].rearrange("p (h a c) -> p h a c", h=8, a=16)
                yield self.op("dve", "tensor_tensor", allV, ["cand"], out=c4,
                        in0=V4[:, :, 0, :].unsqueeze(3).to_broadcast([128, 8, 16, 16]),
                        in1=V4[:, :, 1, :].unsqueeze(2).to_broadcast([128, 8, 16, 16]), op=ALU.add)
                yield self.op("dve", "tensor_tensor", ["I16f"], ["cidx"], out=x4,
                        in0=I4[:, :, 0, :].unsqueeze(3).to_broadcast([128, 8, 16, 16]),
                        in1=I4[:, :, 1, :].unsqueeze(2).to_broadcast([128, 8, 16, 16]), op=ALU.add)
                for h in range(8):
                    hs = slice(h * 256, (h + 1) * 256)
                    lo = slice(h * 16, h * 16 + 8)
                    hi = slice(h * 16 + 8, h * 16 + 16)
                    yield self.op("dve", "max", ["cand"], [("best", h)], out=best[:, lo], in_=cand[:, hs])
                    yield self.op("dve", "max_index", ["cand", ("best", h)], [("J", h)], out=J[:, lo],
                            in_max=best[:, lo], in_values=cand[:, hs])
                    yield self.op("dve", "match_replace", ["cand", ("best", h)], [("cand2", h)], out=cand2[:, hs],
                            in_to_replace=best[:, lo], in_values=cand[:, hs], imm_value=NEG)
                    yield self.op("dve", "max", [("cand2", h)], [("best", h)], out=best[:, hi], in_=cand2[:, hs])
                    yield self.op("dve", "max_index", [("cand2", h), ("best", h)], [("J", h)], out=J[:, hi],
                            in_max=best[:, hi], in_values=cand2[:, hs])
                allB = [("best", h) for h in range(8)]
                allJ = [("J", h) for h in range(8)]
                yield self.op("dve", "tensor_copy", allJ, ["Jf"], out=Jf[:], in_=J[:])
                for r in range(128):
                    h = r // 16
                    yield self.op("dve", "scalar_tensor_tensor", ["iota", "Jf", "cidx"], ["junk2", ("idxf", r)],
                            out=junk[:, 0:256], in0=iota[:], scalar=Jf[:, r:r + 1],
                            in1=cidx[:, h * 256:(h + 1) * 256], op0=ALU.is_equal, op1=ALU.mult,
                            accum_out=idxf[:, r:r + 1])
                allF = [("idxf", r) for r in range(128)]
                yield self.op("dve", "tensor_scalar", allF, [kidx], out=idx[par][:], in0=idxf[:],
                        scalar1=float(l * NEXP), scalar2=None, op0=ALU.add)
                b3, e3, g3 = v3(best, 8, 16), v3(ex, 8, 16), v3(gte[par], 8, 16)
                yield self.op("dve", "tensor_tensor", allB, ["ex"], out=e3, in0=b3,
                        in1=b3[:, :, 0:1].to_broadcast([128, 8, 16]), op=ALU.subtract)
                yield self.op("act", "activation", ["ex"], ["ex"], out=ex[:], in_=ex[:], func=AF.Exp)
                yield self.op("dve", "reduce_sum", ["ex"], ["Z"], out=Z[:], in_=e3, axis=AX.X)
                yield self.op("dve", "reciprocal", ["Z"], ["Z"], out=Z[:], in_=Z[:])
                yield self.op("dve", "tensor_tensor", ["ex", "Z"], [("gte", par)], out=g3, in0=e3,
                        in1=Z[:, :].unsqueeze(2).to_broadcast([128, 8, 16]), op=ALU.mult)

            def ustep(i, r):
                par = i % 3
                kS, ka2, kx1, kidx, kw, kacc = keys(i)
                if True:
                    s_ = r % NS
                    self.s.add("pool", (lambda e, o=ub[s_], ix=idx[par], r=r: e.indirect_dma_start(
                        out=o[:], out_offset=None, in_=utab,
                        in_offset=bass.IndirectOffsetOnAxis(ap=ix[:, r:r + 1], axis=0))),
                        [kidx], [("ub", s_)], dma=True, slot=s_)
                    self.op("dve", "scalar_tensor_tensor", [("ub", s_), "a2p"], ["junk", ("araw", r)],
                            out=junk[:], in0=ub[s_][:], scalar=1.0, in1=a2p[:], op0=ALU.mult,
                            op1=ALU.mult, accum_out=araw[:, r:r + 1])

            def gelu(i):
                par = i % 3
                kS, ka2, kx1, kidx, kw, kacc = keys(i)
                allA = [("araw", r) for r in range(128)]
                self.op("dve", "tensor_tensor", allA, ["t1"], out=t1[:], in0=araw[:], in1=araw[:], op=ALU.mult)
                self.op("dve", "tensor_scalar", ["t1"], ["t1"], out=t1[:], in0=t1[:], scalar1=0.044715,
                        scalar2=1.0, op0=ALU.mult, op1=ALU.add)
                self.op("dve", "tensor_tensor", ["t1"] + allA, ["t1"], out=t1[:], in0=t1[:], in1=araw[:],
                        op=ALU.mult)
                self.op("act", "activation", ["t1"], ["t2"], out=t2[:], in_=t1[:], func=AF.Tanh, scale=C0)
                self.op("dve", "tensor_scalar", ["t2"], ["t2"], out=t2[:], in0=t2[:], scalar1=1.0,
                        scalar2=0.5, op0=ALU.add, op1=ALU.mult)
                self.op("dve", "tensor_tensor", ["t2"] + allA, ["t2"], out=t2[:], in0=t2[:], in1=araw[:],
                        op=ALU.mult)
                self.op("dve", "tensor_tensor", ["t2", ("gte", par)], [kw], out=wgt[par][:], in0=t2[:],
                        in1=gte[par][:], op=ALU.mult)

            def vstep(i, r):
                par = i % 3
                kS, ka2, kx1, kidx, kw, kacc = keys(i)
                if True:
                    s_ = r % NS
                    g_ = r % 4
                    self.s.add("pool", (lambda e, o=vb[s_], ix=idx[par], r=r: e.indirect_dma_start(
                        out=o[:], out_offset=None, in_=vtab,
                        in_offset=bass.IndirectOffsetOnAxis(ap=ix[:, r:r + 1], axis=0))),
                        [kidx], [("vb", s_)], dma=True, slot=NS + s_)
                    self.op("act", "activation", ["pident", kw], [("dg", g_)], out=dg[g_][:], in_=ident[:],
                            func=AF.Copy, scale=wgt[par][:, r:r + 1])
                    for n in range(2):
                        ns = slice(n * 512, (n + 1) * 512)
                        self.op("pe", "matmul", [("dg", g_), ("vb", s_)], [("accp", i % 2)], accp[i % 2][:, ns],
                                dg[g_][:], vb[s_][:, ns], start=(r == 0), stop=(r == 127))

            def final(i):
                b, tl = tiles[i]
                par = i % 3
                rows = slice(tl * 128, (tl + 1) * 128)
                kS, ka2, kx1, kidx, kw, kacc = keys(i)
                self.op("dve", "tensor_tensor", [("accp", i % 2), "g5"], [kacc], out=acc[par][:],
                        in0=accp[i % 2][:], in1=g5[who_par[i]][:], op=ALU.mult)
                self.op("dve", "tensor_tensor", [kacc, kx1], [kacc], out=acc[par][:], in0=acc[par][:],
                        in1=x1[par][:], op=ALU.add)
                if final_:
                    lrow = slice((tl - NCT) * 128, (tl - NCT + 1) * 128)
                    self.dma("sp", d["y"][b, lrow, :], acc[par][:], [kacc], [("y", b, tl)])
                else:
                    self.dma("sp", d["xs"][b, rows, :], acc[par][:], [kacc], [("xs", b, tl)])

            def body():
                self.dma("sp", iota[:], d["iota256"][:, :], [], ["iota"])
                self.dma("sp", ident[:], d["ident"][:, :], [], ["pident"])
                cur = None
                nwho = 0
                for i, (b, tl) in enumerate(tiles):
                    who = 2 if tl < NCT else b
                    if who != cur:
                        cur = who
                        nwho += 1
                        who_first[i] = (who, (nwho - 1) % 2)
                    who_par[i] = (nwho - 1) % 2
                def maybe_g5(i):
                    if i in who_first:
                        who, gp_ = who_first[i]
                        self.load_mod(g5[gp_], l, who, 5, "g5")
                maybe_g5(0)
                loads(0)
                for _ in topk(0):
                    pass
                for i in range(n_t + 1):
                    gen = None
                    if i + 1 < n_t:
                        maybe_g5(i + 1)
                        loads(i + 1)
                        gen = topk(i + 1)
                    if i < n_t:
                        self.op("act", "copy", [("pa2", i % 3)], ["a2p"], out=a2p[:], in_=a2[i % 3][:])
                    for r in range(128):
                        if i < n_t:
                            ustep(i, r)
                        if i > 0:
                            vstep(i - 1, r)
                        if gen is not None:
                            for _ in range(3):
                                if next(gen, "done") == "done":
                                    gen = None
                                    break
                    if i < n_t:
                        gelu(i)
                    if i > 0:
                        final(i - 1)
                    if gen is not None:
                        for _ in gen:
                            pass
            self.phase(body)


def build_program(cfg):
    from contextlib import ExitStack
    nc = bass.Bass("TRN2", target_bir_lowering=False)
    B = Builder(nc, cfg)
    B.din("xin", [NB, T, D])
    B.din("cT3", [128, 8, 3])
    B.din("w_mod", [DEPTH, D, 6 * D])
    B.din("b_mod", [DEPTH, 6 * D])
    B.din("g_mix", [DEPTH, D])
    B.din("g_ffn", [DEPTH, D])
    B.din("w_out_even", [2, D, D])
    B.din("w_out_odd", [2, D, D])
    B.din("peer_wq", [DEPTH, D, D])
    B.din("kbd", [DEPTH, 128, 8, 256])
    B.din("peer_u", [DEPTH, NEXP, D])
    B.din("peer_v", [DEPTH, NEXP, D])
    B.din("ident", [128, 128])
    B.din("iota256", [128, 256])
    B.din("cos64", [SEQ, 64])
    B.din("sin64", [SEQ, 64])
    B.din("tri4", [128, 4, 128])
    B.din("w_in_even", [2, D, 3104])
    B.din("w_in_odd", [2, D, 1536])
    for nm, shp in (("a_q_gain", [2, 64]), ("a_k_gain", [2, 64]), ("a_lambda", [2, 4, 64]), ("a_subln", [2, 128]),
                    ("b_w_af", [2, 16, 256]), ("b_b_af", [2, 256]), ("b_w_ab", [2, 16, 256]), ("b_b_ab", [2, 256]),
                    ("b_gain", [2, 128]), ("c_q_gain", [2, 64]), ("c_k_gain", [2, 64])):
        B.din(nm, shp)
    B.dscr("QT", [NB, 16, 64, T])
    B.dscr("KT", [NB, 8, 64, T])
    B.dscr("V", [NB, T, 512])
    B.dscr("G", [NB, T, 2048])
    if cfg.get("mx_input"):
        B.din("Mx", [NB, T, D])
    else:
        B.dscr("Mx", [NB, T, D])
    B.dout("y", [NB, SEQ, D])
    B.dscr("xs", [NB, T, D])
    B.dscr("A2", [NB, T, D])
    B.dscr("Sc", [NB, T, 2048])
    B.dscr("modD", [DEPTH, 3, 6 * D])
    with ExitStack() as st:
        B.esem = {e: st.enter_context(nc.semaphore(f"es_{e}")) for e in Sched.ENGS}
        B.dsem = {"sp": [st.enter_context(nc.semaphore(f"ds_sp{i}")) for i in range(8)],
                  "act": [st.enter_context(nc.semaphore(f"ds_act{i}")) for i in range(8)],
                  "pool": [st.enter_context(nc.semaphore(f"ds_pool{i}")) for i in range(16)],
                  "pe": [], "dve": []}
        cfg["program"](B)
    return nc


def full_program(B, layers=range(DEPTH), bs=range(NB), skip_p4=(), skip=()):
    B.phase_mod(layers)
    alltiles = [(b, tl) for b in bs for tl in range(NT)]
    for l in layers:
        last = l == DEPTH - 1
        xsrc = "xin" if l == 0 else "xs"
        B.phase_p1(l, alltiles, xsrc)
        if "attn" not in skip:
            B.phase_attn(l, list(bs), not last)
        if l % 2 == 0 and "gla" not in skip:
            B.phase_gla(l, list(bs))
        tiles = [(b, tl) for (b, tl) in alltiles if not (last and tl < NCT)]
        B.phase_p3(l, tiles, xsrc)
        if l not in skip_p4:
            B.phase_p4(l, tiles, last)


def prep_consts():
    ident = np.eye(128, dtype=np.float32)
    iota = np.tile(np.arange(256, dtype=np.float32)[None, :], (128, 1))
    pos = np.arange(SEQ)
    rc = np.stack([(pos // GRID_W).astype(np.float32), (pos % GRID_W).astype(np.float32)], axis=-1)
    inv = (np.float32(10000.0) ** (-np.arange(16, dtype=np.float32) / np.float32(16))).astype(np.float32)
    ang = (rc[:, :, None] * inv[None, None, :]).astype(np.float32)
    c, s_ = np.cos(ang).astype(np.float32), np.sin(ang).astype(np.float32)
    cos64 = np.stack([c, c], axis=2).reshape(SEQ, 64)
    sin64 = np.stack([-s_, s_], axis=2).reshape(SEQ, 64)
    sp, cc = np.meshgrid(np.arange(128), np.arange(128), indexing="ij")
    tri4 = np.stack([sp <= cc, sp > cc, sp >= cc, sp < cc], axis=1).astype(np.float32)
    return {"ident": ident, "iota256": iota, "cos64": np.ascontiguousarray(cos64),
            "sin64": np.ascontiguousarray(sin64), "tri4": np.ascontiguousarray(tri4)}


def prep_core_inputs(inp, core):
    bs = slice(core * NB, (core + 1) * NB)
    xin = np.concatenate([inp["ctx"][bs], inp["x"][bs]], axis=1).astype(np.float32)
    cv = np.stack([inp["c"][core * NB + 0], inp["c"][core * NB + 1], inp["c_ctx"]], axis=1)
    cT3 = np.ascontiguousarray(cv.reshape(8, 128, 3).transpose(1, 0, 2)).astype(np.float32)
    keys = inp["peer_keys"]
    kbd = np.zeros((DEPTH, 128, 8, 256), np.float32)
    kbd[:, 0:64, :, 0:128] = keys[:, :, 0].transpose(0, 3, 1, 2)
    kbd[:, 64:128, :, 128:256] = keys[:, :, 1].transpose(0, 3, 1, 2)
    m = {"xin": np.ascontiguousarray(xin), "cT3": cT3, "kbd": kbd}
    for k in ("w_mod", "b_mod", "g_mix", "g_ffn", "w_out_even", "w_out_odd", "peer_wq", "peer_u", "peer_v",
              "w_in_even", "w_in_odd", "a_q_gain", "a_k_gain", "a_lambda", "a_subln", "b_w_af", "b_b_af",
              "b_w_ab", "b_b_ab", "b_gain", "c_q_gain", "c_k_gain"):
        m[k] = np.ascontiguousarray(inp[k], dtype=np.float32)
    m.update(prep_consts())
    return m


def phase_p1(self, l, tiles, xsrc):
    from contextlib import ExitStack
    d = self.dram
    even = (l % 2 == 0)
    i2 = l // 2
    if even:
        w_in = d["w_in_even"][i2]; ncols = 3104
        qcols, nqh, kcol0, nkh = 0, 8, 512, 8
        vcol0, vw = 1024, 512
        qg_src, kg_src = d["a_q_gain"], d["a_k_gain"]
    else:
        w_in = d["w_in_odd"][i2]; ncols = 1536
        qcols, nqh, kcol0, nkh = 0, 16, 1024, 4
        vcol0, vw = 1280, 256
        qg_src, kg_src = d["c_q_gain"], d["c_k_gain"]
    nh = nqh + nkh
    nchunk = (ncols + 511) // 512
    with ExitStack() as st:
        win = self.sb(st, "win", [128, 8, ncols])
        ident = self.sb(st, "ident", [128, 128])
        shm = self.sb(st, "shm", [128, D])
        gsm = self.sb(st, "gsm", [128, D])
        gain = self.sb(st, "gain", [128, nh, 64])
        cos = [self.sb(st, f"cos{i}", [128, 64]) for i in range(2)]
        sin = [self.sb(st, f"sin{i}", [128, 64]) for i in range(2)]
        xt = [self.sb(st, f"xt{i}", [128, D]) for i in range(2)]
        a = self.sb(st, "a", [128, D])
        aT = self.sb(st, "aT", [128, D])
        P = self.sb(st, "P", [128, ncols])
        sq = self.sb(st, "sq", [128, nh * 64])
        qs = self.sb(st, "qs", [128, nh * 64])
        ssq = self.sb(st, "ssq", [128, nh])
        ss = self.sb(st, "ss", [128, 2])
        junk = self.sb(st, "junk", [128, D])
        qkT = [self.sb(st, f"qkT{i}", [64, nh, 128]) for i in range(2)]
        tp = self.ps(st, "tp", [128, D])
        pp = [self.ps(st, f"pp{i}", [128, 512]) for i in range(2)]
        hp = [self.ps(st, f"hp{i}", [64, 4, 128]) for i in range(2)]
        if even:
            G = [self.sb(st, f"G{i}", [128, 2048]) for i in range(2)]
            wg = self.sb(st, "wg", [32, 512])
            bg = self.sb(st, "bg", [128, 512])
            pfT = self.sb(st, "pfT", [32, 128])
            gp = self.ps(st, "gp", [128, 512])

        def body():
            for k in range(8):
                self.dma("sp", P[:, :], w_in[k * 128:(k + 1) * 128, :], [], [("P", n) for n in range(nchunk)])
                self.op("dve", "tensor_copy", [("P", n) for n in range(nchunk)], ["win"], out=_r(win[:, k, :]),
                        in_=P[:, :])
            self.dma("act", ident[:], d["ident"][:, :], [], ["ident"])
            for h in range(nh):
                src = (qg_src if h < nqh else kg_src)[i2:i2 + 1, :].to_broadcast([128, 64])
                self.dma("act", gain[:, h, :], src, [], ["gain"])
            self.op("dve", "tensor_scalar", ["gain"], ["gain"], out=gain[:, 0:nqh, :], in0=gain[:, 0:nqh, :],
                    scalar1=0.125, scalar2=None, op0=ALU.mult)
            if even:
                self.op("dve", "memset", [], ["wg"], wg[:], 0.0)
                self.dma("act", wg[0:16, 0:256], d["b_w_af"][i2], ["wg"], ["wg"])
                self.dma("act", wg[16:32, 256:512], d["b_w_ab"][i2], ["wg"], ["wg"])
                self.dma("act", bg[:, 0:256], d["b_b_af"][i2:i2 + 1, :].to_broadcast([128, 256]), [], ["bg"])
                self.dma("act", bg[:, 256:512], d["b_b_ab"][i2:i2 + 1, :].to_broadcast([128, 256]), [], ["bg"])
            cur_who = None
            for i, (b, tl) in enumerate(tiles):
                par = i % 2
                who = 2 if tl < NCT else b
                rows = slice(tl * 128, (tl + 1) * 128)
                latent = tl >= NCT
                if who != cur_who:
                    cur_who = who
                    self.load_mod(shm, l, who, 0, "shm")
                    self.load_mod(gsm, l, who, 1, "gsm")
                kx = ("xt", par)
                self.dma("sp", xt[par][:], d[xsrc][b, rows, :], [("xs", b, tl)], [kx])
                if latent:
                    lr = slice((tl - NCT) * 128, (tl - NCT + 1) * 128)
                    self.dma("act", cos[par][:], d["cos64"][lr, :], [], [("cos", par)])
                    self.dma("act", sin[par][:], d["sin64"][lr, :], [], [("sin", par)])
                self.op("act", "activation", [kx], ["junk", "ss"], out=junk[:], in_=xt[par][:], func=AF.Square,
                        accum_out=ss[:, 0:1])
                self.op("dve", "tensor_scalar", ["ss"], ["ss"], out=ss[:, 0:1], in0=ss[:, 0:1],
                        scalar1=1.0 / D, scalar2=EPS, op0=ALU.mult, op1=ALU.add)
                self.op("act", "activation", ["ss"], ["ss"], out=ss[:, 0:1], in_=ss[:, 0:1], func=AF.Sqrt)
                self.op("dve", "reciprocal", ["ss"], ["ss"], out=ss[:, 0:1], in_=ss[:, 0:1])
                self.op("dve", "scalar_tensor_tensor", [kx, "ss", "gsm"], ["a"], out=a[:], in0=xt[par][:],
                        scalar=ss[:, 0:1], in1=gsm[:], op0=ALU.mult, op1=ALU.mult)
                self.op("dve", "tensor_tensor", ["a", "shm"], ["a"], out=a[:], in0=a[:], in1=shm[:], op=ALU.add)
                for k in range(8):
                    cs = slice(k * 128, (k + 1) * 128)
                    self.op("pe", "transpose", ["a", "ident"], ["tp"], out=tp[:, cs], in_=a[:, cs],
                            identity=ident[:])
                self.op("dve", "tensor_copy", ["tp"], ["aT"], out=_r(aT[:]), in_=tp[:])
                for n in range(nchunk):
                    c0, c1 = n * 512, min(ncols, (n + 1) * 512)
                    pb = n % 2
                    for k in range(8):
                        self.op("pe", "matmul", ["aT", "win"], [("pp", pb)], pp[pb][:, 0:c1 - c0],
                                _r(aT[:, k * 128:(k + 1) * 128]), _r(win[:, k, c0:c1]), start=(k == 0), stop=(k == 7))
                    if n % 2 == 0:
                        self.op("act", "copy", [("pp", pb)], [("P", n)], out=P[:, c0:c1], in_=pp[pb][:, 0:c1 - c0])
                    else:
                        self.op("dve", "tensor_copy", [("pp", pb)], [("P", n)], out=P[:, c0:c1],
                                in_=pp[pb][:, 0:c1 - c0])
                allP = [("P", n) for n in range(nchunk)]
                qk = [(qcols, nqh, 0), (kcol0, nkh, nqh)]
                for (c0, n_, h0) in qk:
                    w_ = n_ * 64
                    src3 = P[:, c0:c0 + w_].rearrange("p (h e) -> p h e", h=n_)
                    sq3 = sq[:, h0 * 64:h0 * 64 + w_].rearrange("p (h e) -> p h e", h=n_)
                    self.op("dve", "tensor_tensor", allP, [("sq", h0)], out=sq3, in0=src3, in1=src3, op=ALU.mult)
                    self.op("dve", "reduce_sum", [("sq", h0)], [("ssq", h0)], out=ssq[:, h0:h0 + n_], in_=sq3,
                            axis=AX.X)
                    self.op("dve", "tensor_scalar", [("ssq", h0)], [("ssq", h0)], out=ssq[:, h0:h0 + n_],
                            in0=ssq[:, h0:h0 + n_], scalar1=1.0 / 64, scalar2=EPS, op0=ALU.mult, op1=ALU.add)
                    self.op("act", "activation", [("ssq", h0)], [("ssq", h0)], out=ssq[:, h0:h0 + n_],
                            in_=ssq[:, h0:h0 + n_], func=AF.Sqrt)
                    self.op("dve", "reciprocal", [("ssq", h0)], [("ssq", h0)], out=ssq[:, h0:h0 + n_],
                            in_=ssq[:, h0:h0 + n_])
                    self.op("dve", "tensor_tensor", allP + [("ssq", h0)], [("sq", h0)], out=sq3, in0=src3,
                            in1=ssq[:, h0:h0 + n_].unsqueeze(2).to_broadcast([128, n_, 64]), op=ALU.mult)
                    self.op("dve", "tensor_tensor", [("sq", h0), "gain"], [("sq", h0)], out=sq3, in0=sq3,
                            in1=gain[:, h0:h0 + n_, :], op=ALU.mult)
                    if latent:
                        x4 = sq[:, h0 * 64:h0 * 64 + w_].rearrange("p (g t f) -> p g t f", t=2, f=16)
                        s4 = qs[:, h0 * 64:h0 * 64 + w_].rearrange("p (g t f) -> p g t f", t=2, f=16)
                        qs3 = qs[:, h0 * 64:h0 * 64 + w_].rearrange("p (h e) -> p h e", h=n_)
                        self.op("dve", "tensor_copy", [("sq", h0)], [("qs", h0)], out=s4[:, :, 0, :], in_=x4[:, :, 1, :])
                        self.op("dve", "tensor_copy", [("sq", h0)], [("qs", h0)], out=s4[:, :, 1, :], in_=x4[:, :, 0, :])
                        self.op("dve", "tensor_tensor", [("sq", h0), ("cos", par)], [("sq", h0)], out=sq3, in0=sq3,
                                in1=cos[par][:, :].unsqueeze(1).to_broadcast([128, n_, 64]), op=ALU.mult)
                        self.op("dve", "tensor_tensor", [("qs", h0), ("sin", par)], [("qs", h0)], out=qs3, in0=qs3,
                                in1=sin[par][:, :].unsqueeze(1).to_broadcast([128, n_, 64]), op=ALU.mult)
                        self.op("dve", "tensor_tensor", [("sq", h0), ("qs", h0)], [("sq", h0)], out=sq3, in0=sq3,
                                in1=qs3, op=ALU.add)
                kq = ("qkT", par)
                for g4 in range(nh // 4):
                    hb = g4 % 2
                    for j in range(4):
                        h = g4 * 4 + j
                        h0 = 0 if h < nqh else nqh
                        self.op("pe", "transpose", [("sq", h0), "ident"], [("hp", hb)], out=hp[hb][:, j, :],
                                in_=sq[:, h * 64:(h + 1) * 64], identity=ident[:])
                    if g4 % 2 == 0:
                        self.op("act", "copy", [("hp", hb)], [kq], out=qkT[par][:, g4 * 4:g4 * 4 + 4, :], in_=hp[hb][:])
                    else:
                        self.op("dve", "tensor_copy", [("hp", hb)], [kq], out=qkT[par][:, g4 * 4:g4 * 4 + 4, :],
                                in_=hp[hb][:])
                self.dma("sp", d["QT"][b, 0:nqh, :, rows].rearrange("h e t -> e h t"), qkT[par][:, 0:nqh, :], [kq],
                         [("QT", b, tl)])
                self.dma("sp", d["KT"][b, 0:nkh, :, rows].rearrange("h e t -> e h t"), qkT[par][:, nqh:nh, :], [kq],
                         [("KT", b, tl)])
                self.dma("act", d["V"][b, rows, 0:vw], P[:, vcol0:vcol0 + vw], allP, [("V", b, tl)])
                if even:
                    Gp = G[par]
                    kG = ("G", par)
                    self.op("dve", "tensor_scalar", allP, [kG], out=Gp[:, 0:256], in0=P[:, 1536:1792],
                            scalar1=0.125, scalar2=None, op0=ALU.mult)
                    self.op("act", "copy", allP, [kG], out=Gp[:, 256:1536], in_=P[:, 1792:3072])
                    self.op("pe", "transpose", allP + ["ident"], [("hp", 0)], out=hp[0][0:32, 0, :],
                            in_=P[:, 3072:3104], identity=ident[:])
                    self.op("act", "copy", [("hp", 0)], ["pfT"], out=pfT[:], in_=hp[0][0:32, 0, :])
                    self.op("pe", "matmul", ["pfT", "wg"], ["gp"], gp[:], pfT[:], wg[:], start=True, stop=True)
                    self.op("dve", "tensor_tensor", ["gp", "bg"], [kG], out=Gp[:, 1536:2048], in0=gp[:], in1=bg[:],
                            op=ALU.add)
                    self.op("act", "activation", [kG], [kG], out=Gp[:, 1536:2048], in_=Gp[:, 1536:2048],
                            func=AF.Exp, scale=-1.0)
                    self.op("act", "activation", [kG], [kG], out=Gp[:, 1536:2048], in_=Gp[:, 1536:2048],
                            func=AF.Ln, bias=1.0)
                    self.op("dve", "tensor_scalar", [kG], [kG], out=Gp[:, 1536:2048], in0=Gp[:, 1536:2048],
                            scalar1=-1.0 / 16.0, scalar2=None, op0=ALU.mult)
                    self.dma("sp", d["G"][b, rows, :], Gp[:], [kG], [("G", b, tl)])
        self.phase(body)


Builder.phase_p1 = phase_p1


def phase_attn(self, l, bs, need_ctx):
    from contextlib import ExitStack
    d = self.dram
    even = (l % 2 == 0)
    i2 = l // 2
    dv = 128 if even else 64
    lam_init = 0.8 - 0.6 * math.exp(-0.3 * l)
    with ExitStack() as st:
        KTs = [[self.sb(st, f"KT{i}{u}", [64, T]) for u in range(2)] for i in range(2)]
        Vp = [self.sb(st, f"Vp{i}", [128, NT, dv + 2], BF16) for i in range(2)]
        QTs = [[self.sb(st, f"QTs{i}{u}", [64, 512]) for u in range(2)] for i in range(2)]
        PT = [self.sb(st, f"PT{i}", [128, 512], BF16) for i in range(3)]
        Kstg = self.sb(st, "Kstg", [64, T])
        Vstg = self.sb(st, "Vstg", [128, NT, dv])
        Qstg = [self.sb(st, f"Qstg{i}", [64, 512]) for i in range(2)]
        rz = self.sb(st, "rz", [128, 2])
        tmp = self.sb(st, "tmp", [128, 128])
        ob = [self.sb(st, f"ob{i}", [128, 128]) for i in range(2)]
        junk = self.sb(st, "junk", [128, 128])
        ss = self.sb(st, "ss", [128, 2])
        Sp = [self.ps(st, f"Sp{i}", [128, 512]) for i in range(2)]
        O = self.ps(st, "O", [128, 6, 512])
        Asb = self.sb(st, "Asb", [128, 4, 128])
        if even:
            lt = self.sb(st, "lt", [128, 4, 64])
            lp = self.sb(st, "lp", [128, 2, 64])
            ls = self.sb(st, "ls", [128, 2])
            nlam = self.sb(st, "nlam", [128, 1])
            sg = self.sb(st, "sg", [128, 128])

        def body():
            for i in range(2):
                self.op("dve", "memset", [], [("Vp", i)], Vp[i][:, :, dv:dv + 2], 1.0)
            if even:
                self.dma("sp", lt[:], d["a_lambda"][i2:i2 + 1, :, :].to_broadcast([128, 4, 64]), [], ["lt"])
                self.dma("sp", sg[:], d["a_subln"][i2:i2 + 1, :].to_broadcast([128, 128]), [], ["sg"])
                self.op("dve", "tensor_scalar", ["sg"], ["sg"], out=sg[:], in0=sg[:], scalar1=1.0 - lam_init,
                        scalar2=None, op0=ALU.mult)
                self.op("dve", "tensor_tensor", ["lt"], ["lp"], out=lp[:, 0, :], in0=lt[:, 0, :], in1=lt[:, 1, :],
                        op=ALU.mult)
                self.op("dve", "tensor_tensor", ["lt"], ["lp"], out=lp[:, 1, :], in0=lt[:, 2, :], in1=lt[:, 3, :],
                        op=ALU.mult)
                self.op("dve", "reduce_sum", ["lp"], ["ls"], out=ls[:], in_=lp[:], axis=AX.X)
                self.op("act", "activation", ["ls"], ["ls"], out=ls[:], in_=ls[:], func=AF.Exp)
                self.op("dve", "tensor_tensor", ["ls"], ["nlam"], out=nlam[:], in0=ls[:, 1:2], in1=ls[:, 0:1],
                        op=ALU.subtract)
                self.op("dve", "tensor_scalar", ["nlam"], ["nlam"], out=nlam[:], in0=nlam[:], scalar1=-lam_init,
                        scalar2=None, op0=ALU.add)
            rnd = [0]
            sidx = [0]
            oslot = [0]
            qidx = 0
            for b in bs:
                for kvh in range(4):
                    vpar = (b * 4 + kvh) % 2
                    kV = ("Vp", vpar)
                    self.dma("sp", Vstg[:],
                             d["V"][b, :, kvh * dv:(kvh + 1) * dv].rearrange("(t p) e -> p t e", p=128),
                             [("V", b, t_) for t_ in range(NT)], ["Vstg"])
                    self.op("dve", "tensor_copy", ["Vstg"], [kV], out=Vp[vpar][:, :, 0:dv], in_=Vstg[:])
                    if even:
                        rounds = [[(2 * kvh, 2 * kvh), (2 * kvh + 1, 2 * kvh + 1)]]
                    else:
                        rounds = [[(kvh * 4 + 0, kvh), (kvh * 4 + 1, kvh)], [(kvh * 4 + 2, kvh), (kvh * 4 + 3, kvh)]]
                    kpar = (b * 4 + kvh) % 2
                    kheads = sorted(set(kh for r_ in rounds for (_, kh) in r_))
                    kmap = {}
                    for u, kh in enumerate(kheads):
                        self.dma("act", Kstg[:], d["KT"][b, kh, :, :], [("KT", b, t_) for t_ in range(NT)], ["Kstg"])
                        self.op("dve", "tensor_copy", ["Kstg"], [("KTs", kpar, u)], out=_r(KTs[kpar][u][:]),
                                in_=Kstg[:])
                        kmap[kh] = u
                    for units in rounds:
                        blocks = [(NCT * 128 + j * 512, 512, list(range(NT))) for j in range(SEQ // 512)]
                        if need_ctx:
                            blocks = [(0, CTX, list(range(NCT)))] + blocks
                        for (t0, nq, kts) in blocks:
                            nqs = nq // 128
                            qpar = qidx % 2
                            qidx += 1
                            for u, (qh, kh) in enumerate(units):
                                self.dma("sp", Qstg[u][:, 0:nq], d["QT"][b, qh, :, t0:t0 + nq],
                                         [("QT", b, t_) for t_ in range(t0 // 128, (t0 + nq) // 128)],
                                         [("Qstg", u)])
                                self.op("dve", "tensor_copy", [("Qstg", u)], [("QTs", qpar, u)],
                                        out=_r(QTs[qpar][u][:, 0:nq]), in_=Qstg[u][:, 0:nq])
                            for u, (qh, kh) in enumerate(units):
                                slots = [(oslot[0] + q_) % 6 for q_ in range(nqs)]
                                oslot[0] += nqs
                                ku = kmap[kh]
                                for ki, kt in enumerate(kts):
                                    sb_ = sidx[0] % 2
                                    sidx[0] += 1
                                    pb_ = sidx[0] % 3
                                    self.op("pe", "matmul", [("KTs", kpar, ku), ("QTs", qpar, u)], [("Sp", sb_)],
                                            Sp[sb_][:, 0:nq], _r(KTs[kpar][ku][:, kt * 128:(kt + 1) * 128]),
                                            _r(QTs[qpar][u][:, 0:nq]), start=True, stop=True)
                                    self.op("act", "activation", [("Sp", sb_)], [("PT", pb_)], out=PT[pb_][:, 0:nq],
                                            in_=Sp[sb_][:, 0:nq], func=AF.Exp)
                                    for qs_ in range(nqs):
                                        slot = slots[qs_]
                                        self.op("pe", "matmul", [("PT", pb_), kV], [("O", slot)],
                                                O[:, slot, 0:dv + 2], PT[pb_][:, qs_ * 128:(qs_ + 1) * 128],
                                                Vp[vpar][:, kt, :], start=(ki == 0), stop=(ki == len(kts) - 1))
                                for qs_ in range(nqs):
                                    slot = slots[qs_]
                                    tl = t0 // 128 + qs_
                                    rows = slice(tl * 128, (tl + 1) * 128)
                                    self.op("dve", "reciprocal", [("O", slot)], ["rz"], out=rz[:, 0:1],
                                            in_=O[:, slot, dv:dv + 1])
                                    if even and u == 0:
                                        self.op("dve", "tensor_scalar", [("O", slot), "rz"], [("Asb", qs_)],
                                                out=Asb[:, qs_, :], in0=O[:, slot, 0:dv], scalar1=rz[:, 0:1],
                                                scalar2=None, op0=ALU.mult)
                                    elif even:
                                        opar = rnd[0] % 2
                                        rnd[0] += 1
                                        ko = ("ob", opar)
                                        self.op("dve", "tensor_scalar", [("O", slot), "rz", "nlam"], ["tmp"], out=tmp[:],
                                                in0=O[:, slot, 0:dv], scalar1=rz[:, 0:1], scalar2=nlam[:, 0:1],
                                                op0=ALU.mult, op1=ALU.mult)
                                        self.op("dve", "tensor_tensor", ["tmp", ("Asb", qs_)], ["tmp"], out=tmp[:],
                                                in0=tmp[:], in1=Asb[:, qs_, :], op=ALU.add)
                                        self.op("act", "activation", ["tmp"], ["junk", "ss"], out=junk[:], in_=tmp[:],
                                                func=AF.Square, accum_out=ss[:, 0:1])
                                        self.op("dve", "tensor_scalar", ["ss"], ["ss"], out=ss[:, 0:1], in0=ss[:, 0:1],
                                                scalar1=1.0 / 128, scalar2=EPS, op0=ALU.mult, op1=ALU.add)
                                        self.op("act", "activation", ["ss"], ["ss"], out=ss[:, 0:1], in_=ss[:, 0:1],
                                                func=AF.Sqrt)
                                        self.op("dve", "reciprocal", ["ss"], ["ss"], out=ss[:, 0:1], in_=ss[:, 0:1])
                                        self.op("dve", "scalar_tensor_tensor", ["tmp", "ss", "sg"], [ko],
                                                out=ob[opar][:], in0=tmp[:], scalar=ss[:, 0:1], in1=sg[:],
                                                op0=ALU.mult, op1=ALU.mult)
                                        self.dma("sp", d["Mx"][b, rows, kvh * 128:(kvh + 1) * 128], ob[opar][:], [ko],
                                                 [("Mx", b, tl, kvh)])
                                    else:
                                        opar = rnd[0] % 2
                                        rnd[0] += 1
                                        ko = ("ob", opar)
                                        self.op("dve", "tensor_scalar", [("O", slot), "rz"], [ko],
                                                out=ob[opar][:, 0:dv], in0=O[:, slot, 0:dv], scalar1=rz[:, 0:1],
                                                scalar2=None, op0=ALU.mult)
                                        self.dma("sp", d["Mx"][b, rows, qh * 64:(qh + 1) * 64], ob[opar][:, 0:dv],
                                                 [ko], [("Mx", b, tl, qh)])
        self.phase(body)


Builder.phase_attn = phase_attn


def phase_gla(self, l, bs):
    from contextlib import ExitStack
    d = self.dram
    i2 = l // 2
    with ExitStack() as st:
        tri = self.sb(st, "tri", [128, 4, 128])
        ident = self.sb(st, "ident", [128, 128])
        gg = self.sb(st, "gg", [128, 128])
        Gt = [self.sb(st, f"Gt{i}", [128, 2048]) for i in range(2)]
        Of = self.sb(st, "Of", [128, NT, 512])
        ek = self.sb(st, "ek", [128, 256])
        ke = self.sb(st, "ke", [128, 256])
        eT = self.sb(st, "eT", [64, 4, 128])
        eiT = self.sb(st, "eiT", [64, 4, 128])
        qdT = self.sb(st, "qdT", [64, 4, 128])
        kiT = self.sb(st, "kiT", [64, 4, 128])
        Am = [self.sb(st, f"Am{i}", [128, 128]) for i in range(2)]
        H = [self.sb(st, f"H{i}", [64, 4, 128]) for i in range(2)]
        osum = self.sb(st, "osum", [128, 128])
        sr = self.sb(st, "sr", [128, 128])
        gout = [self.sb(st, f"gout{i}", [128, 512]) for i in range(2)]
        junk = self.sb(st, "junk", [128, 128])
        ss = self.sb(st, "ss", [128, 2])
        cr = self.ps(st, "cr", [128, 256])
        ct = self.ps(st, "ct", [64, 4, 128])
        gT = self.ps(st, "gT", [64, 8, 128])
        At = self.ps(st, "At", [128, 2, 128])
        op_ = self.ps(st, "op", [128, 2, 128])
        dS = self.ps(st, "dS", [64, 2, 128])

        def body():
            self.dma("sp", tri[:], d["tri4"][:, :, :], [], ["tri"])
            self.dma("sp", ident[:], d["ident"][:, :], [], ["ident"])
            self.dma("sp", gg[:], d["b_gain"][i2:i2 + 1, :].to_broadcast([128, 128]), [], ["gg"])
            cnt = 0
            hcnt = 0
            for b in bs:
                for dr in range(2):
                    order = list(range(NT)) if dr == 0 else [1, 0] + list(range(NT - 1, NCT - 1, -1))
                    lac = 1536 + 256 * dr
                    incl, strict = tri[:, 2 * dr, :], tri[:, 2 * dr + 1, :]
                    hp_ = cnt % 2
                    self.op("dve", "memset", [], [("H", hp_)], H[hp_][:], 0.0)
                    for tl in order:
                        par = cnt % 2
                        cnt += 1
                        Hc, Hn = H[(cnt - 1) % 2], H[cnt % 2]
                        kHc, kHn = ("H", (cnt - 1) % 2), ("H", cnt % 2)
                        rows = slice(tl * 128, (tl + 1) * 128)
                        G_ = Gt[par]
                        kG = ("Gt", par)
                        self.dma("sp", G_[:], d["G"][b, rows, :], [("G", b, tl)], [kG])
                        la = G_[:, lac:lac + 256]
                        self.op("pe", "matmul", [kG, "tri"], ["cr"], cr[:], strict, la, start=True, stop=True)
                        self.op("act", "activation", ["cr"], ["ek"], out=ek[:], in_=cr[:], func=AF.Exp)
                        self.op("dve", "tensor_tensor", ["ek", kG], ["ke"], out=ke[:], in0=ek[:], in1=G_[:, 256:512],
                                op=ALU.mult)
                        for h in range(4):
                            self.op("pe", "matmul", [kG, "tri"], ["ct"], ct[:, h, :], G_[:, lac + h * 64:lac + (h + 1) * 64],
                                    incl, start=True, stop=True)
                        self.op("act", "activation", ["ct"], ["eT"], out=eT[:], in_=ct[:], func=AF.Exp)
                        self.op("act", "activation", ["ct"], ["eiT"], out=eiT[:], in_=ct[:], func=AF.Exp, scale=-1.0)
                        for h in range(4):
                            self.op("pe", "transpose", [kG, "ident"], ["gT"], out=gT[:, h, :],
                                    in_=G_[:, h * 64:(h + 1) * 64], identity=ident[:])
                            self.op("pe", "transpose", [kG, "ident"], ["gT"], out=gT[:, 4 + h, :],
                                    in_=G_[:, 256 + h * 64:256 + (h + 1) * 64], identity=ident[:])
                        self.op("dve", "tensor_tensor", ["gT", "eT"], ["qdT"], out=qdT[:], in0=gT[:, 0:4, :], in1=eT[:],
                                op=ALU.mult)
                        self.op("dve", "tensor_tensor", ["gT", "eiT"], ["kiT"], out=kiT[:], in0=gT[:, 4:8, :],
                                in1=eiT[:], op=ALU.mult)
                        dcol = 127 if dr == 0 else 0
                        kgo = ("gout", par)
                        for h in range(4):
                            ap_ = hcnt % 2
                            hcnt += 1
                            gv = G_[:, 512 + h * 128:512 + (h + 1) * 128]
                            self.op("pe", "matmul", ["kiT", "qdT"], [("At", ap_)], At[:, ap_, :], kiT[:, h, :],
                                    qdT[:, h, :], start=True, stop=True)
                            self.op("dve", "tensor_tensor", [("At", ap_), "tri"], [("Am", ap_)], out=Am[ap_][:],
                                    in0=At[:, ap_, :], in1=incl, op=ALU.mult)
                            self.op("pe", "matmul", [("Am", ap_), kG], [("op", ap_)], op_[:, ap_, :], Am[ap_][:], gv,
                                    start=True, stop=False)
                            self.op("pe", "matmul", ["qdT", kHc], [("op", ap_)], op_[:, ap_, :], qdT[:, h, :],
                                    Hc[:, h, :], start=False, stop=True)
                            self.op("pe", "matmul", ["ke", kG], [("dS", ap_)], dS[:, ap_, :], ke[:, h * 64:(h + 1) * 64],
                                    gv, start=True, stop=True)
                            self.op("dve", "scalar_tensor_tensor", [kHc, "eT", ("dS", ap_)], [kHn], out=Hn[:, h, :],
                                    in0=Hc[:, h, :], scalar=eT[:, h, dcol:dcol + 1], in1=dS[:, ap_, :],
                                    op0=ALU.mult, op1=ALU.add)
                            hs = slice(h * 128, (h + 1) * 128)
                            if dr == 0:
                                self.op("act", "copy", [("op", ap_)], [("Of", tl)], out=Of[:, tl, hs],
                                        in_=op_[:, ap_, :])
                            else:
                                self.op("dve", "tensor_tensor", [("op", ap_), ("Of", tl)], ["osum"], out=osum[:],
                                        in0=op_[:, ap_, :], in1=Of[:, tl, hs], op=ALU.add)
                                self.op("act", "activation", ["osum"], ["junk", "ss"], out=junk[:], in_=osum[:],
                                        func=AF.Square, accum_out=ss[:, 0:1])
                                self.op("dve", "tensor_scalar", ["ss"], ["ss"], out=ss[:, 0:1], in0=ss[:, 0:1],
                                        scalar1=1.0 / 128, scalar2=EPS, op0=ALU.mult, op1=ALU.add)
                                self.op("act", "activation", ["ss"], ["ss"], out=ss[:, 0:1], in_=ss[:, 0:1],
                                        func=AF.Sqrt)
                                self.op("dve", "reciprocal", ["ss"], ["ss"], out=ss[:, 0:1], in_=ss[:, 0:1])
                                self.op("dve", "scalar_tensor_tensor", ["osum", "ss", "gg"], ["osum"], out=osum[:],
                                        in0=osum[:], scalar=ss[:, 0:1], in1=gg[:], op0=ALU.mult, op1=ALU.mult)
                                self.op("act", "activation", [kG], ["sr"], out=sr[:],
                                        in_=G_[:, 1024 + h * 128:1024 + (h + 1) * 128], func=AF.Silu)
                                self.op("dve", "tensor_tensor", ["osum", "sr"], [kgo], out=gout[par][:, hs],
                                        in0=osum[:], in1=sr[:], op=ALU.mult)
                        if dr == 1:
                            self.dma("sp", d["Mx"][b, rows, 512:1024], gout[par][:], [kgo], [("Mxg", b, tl)])
        self.phase(body)


Builder.phase_gla = phase_gla


N_CORES = 8


def kernel(**inputs):
    inp = {k: np.asarray(v) for k, v in inputs.items()}
    cfg = {"program": full_program}
    nc = build_program(cfg)
    in_maps = [prep_core_inputs(inp, c) for c in range(N_CORES)]
    res = run_bass_kernel_spmd(nc, in_maps, core_ids=list(range(N_CORES)))
    out = np.concatenate([np.asarray(r["y"]) for r in res.results], axis=0)
    return out.astype(np.float32)
```
